# Optimizing a Trainium2 kernel written in Bass

```python
import math
import jax
import jax.numpy as jnp
from jax import lax
import numpy as np

D_MODEL = 1024
BATCH = 16
SEQ = 2048
DEPTH = 1

ATT_PATTERNS = ((128, 1), (512, 4), (2048, 16))
N_ATT_GROUPS = 3
HEADS_PER_GROUP = 4
ATT_HEADS = N_ATT_GROUPS * HEADS_PER_GROUP
HEAD_DIM = 64
ATT_WIDTH = ATT_HEADS * HEAD_DIM
ATT_OUT_WIDTH = HEADS_PER_GROUP * HEAD_DIM
REL_BUCKETS = 32
REL_MAX_DIST = 2048
RWKV_HEADS = 8
RWKV_HEAD_DIM = 64
RWKV_WIDTH = RWKV_HEADS * RWKV_HEAD_DIM
DECAY_LORA = 64
ICLR_LORA = 64
GATE_LORA = 160
RWKV_SHIFT_WIDTH = 3 * RWKV_WIDTH + DECAY_LORA + ICLR_LORA + GATE_LORA
GN_EPS = 64e-5
N_BRANCHES = 2
IN_WIDTH = 3 * ATT_WIDTH + RWKV_SHIFT_WIDTH + N_BRANCHES * D_MODEL
N_EXPERT_GROUPS = 4
EXPERTS_PER_GROUP = 8
N_EXPERTS = N_EXPERT_GROUPS * EXPERTS_PER_GROUP
TOP_K = 2
D_EXPERT = 512
ROW_BLOCK = 256
NORM_EPS = 1e-6
NEG_INF = -1e30

kernel_name = 'hybrid_dilated_rwkv7_hmoe_block'


def rms_norm(x, gain, eps=NORM_EPS):
    xf = x.astype(jnp.float32)
    y = xf * lax.rsqrt(jnp.mean(xf * xf, axis=-1, keepdims=True) + eps)
    return (y * gain.astype(jnp.float32)).astype(x.dtype)


def t5_causal_bucket(dist):
    max_exact = REL_BUCKETS // 2
    d = jnp.maximum(dist.astype(jnp.float32), 1.0)
    large = max_exact + (jnp.log(d / max_exact) / math.log(REL_MAX_DIST / max_exact)
                         * (REL_BUCKETS - max_exact)).astype(jnp.int32)
    large = jnp.minimum(large, REL_BUCKETS - 1)
    return jnp.where(dist < max_exact, dist, large)


def dilated_window_attention(q, k, v, bias_table, window, dilation):
    B, S, H, E = q.shape
    L = S // dilation
    W = window // dilation
    n_blk = -(-L // W)
    Lp = n_blk * W

    def to_sub(t):
        return t.reshape(B, L, dilation, H, E).transpose(0, 2, 3, 1, 4)

    qb = jnp.pad(to_sub(q), ((0, 0),) * 3 + ((0, Lp - L), (0, 0))).reshape(B, dilation, H, n_blk, W, E)
    kv_pad = ((0, 0),) * 3 + ((W, Lp - L), (0, 0))

    def band(t):
        t = jnp.pad(to_sub(t), kv_pad)
        prev = t[:, :, :, :Lp].reshape(B, dilation, H, n_blk, W, E)
        cur = t[:, :, :, W:].reshape(B, dilation, H, n_blk, W, E)
        return jnp.concatenate([prev, cur], axis=4)

    kb, vb = band(k), band(v)
    qi = jnp.arange(W)[:, None]
    kj = jnp.arange(2 * W)[None, :]
    rel = qi + W - kj
    key_pos = (jnp.arange(n_blk) * W - W)[:, None, None] + kj[None]
    valid = (rel >= 0)[None] & (rel <= W)[None] & (key_pos >= 0)
    bias = bias_table[t5_causal_bucket(jnp.clip(rel, 0, W) * dilation)]
    bias = jnp.transpose(bias, (2, 0, 1)).astype(jnp.float32)[:, None]
    s = jnp.einsum('bdhnqe,bdhnke->bdhnqk', qb, kb).astype(jnp.float32) * (E ** -0.5) + bias
    s = jnp.where(valid, s, NEG_INF)
    lse = jax.nn.logsumexp(s, axis=-1)
    p = jnp.exp(s - lse[..., None])
    o = jnp.einsum('bdhnqk,bdhnke->bdhnqe', p.astype(v.dtype), vb)

    def from_sub(t):
        t = t.reshape((B, dilation, H, Lp) + t.shape[5:])[:, :, :, :L]
        t = jnp.moveaxis(t, 3, 1)
        return t.reshape((B, S, H) + t.shape[4:])

    return from_sub(o), from_sub(lse)


def wkv7_scan(r, decay, k, v, kk, a):
    B, S, H, N = r.shape

    def step(state, inp):
        r_t, w_t, k_t, v_t, kk_t, b_t = inp
        sa = jnp.einsum('bhvk,bhk->bhv', state, -kk_t)
        state = (state * w_t[:, :, None, :] + sa[..., None] * b_t[:, :, None, :]
                 + v_t[..., None] * k_t[:, :, None, :])
        return state, jnp.einsum('bhvk,bhk->bhv', state, r_t)

    xs = tuple(jnp.moveaxis(t, 1, 0) for t in (r, decay, k, v, kk, kk * a))
    _, out = lax.scan(step, jnp.zeros((B, H, N, N), jnp.float32), xs)
    return jnp.moveaxis(out, 0, 1)


def rwkv7_time_mix(p, shift_mu, w0, w_up, a0, a_up, g_up, k_k, k_a, r_k, ln_w, ln_b, w_rwkv_out):
    B, S, _ = p.shape
    H, N, C = RWKV_HEADS, RWKV_HEAD_DIM, RWKV_WIDTH
    pf = p.astype(jnp.float32)
    prev = jnp.pad(pf, ((0, 0), (1, 0), (0, 0)))[:, :-1]
    pm = pf + (prev - pf) * shift_mu
    r, k, v, xw, xa, xg = jnp.split(pm, [C, 2 * C, 3 * C, 3 * C + DECAY_LORA,
                                          3 * C + DECAY_LORA + ICLR_LORA], axis=-1)
    w_log = -jax.nn.softplus(-(w0 + jnp.tanh(xw) @ w_up)) - 0.5
    decay = jnp.exp(-jnp.exp(w_log))
    a = jax.nn.sigmoid(a0 + xa @ a_up)
    g = jax.nn.sigmoid(xg) @ g_up
    hd = lambda t: t.reshape(B, S, H, N)
    kk = hd(k * k_k)
    kk = kk / jnp.maximum(jnp.sqrt(jnp.sum(kk * kk, axis=-1, keepdims=True)), 1e-12)
    k = k * (1.0 + (a - 1.0) * k_a)
    o = wkv7_scan(hd(r), hd(decay), hd(k), hd(v), kk, hd(a))
    mu = jnp.mean(o, axis=-1, keepdims=True)
    var = jnp.mean(jnp.square(o - mu), axis=-1, keepdims=True)
    o = ((o - mu) * lax.rsqrt(var + GN_EPS)).reshape(B, S, C) * ln_w + ln_b
    bonus = jnp.sum(hd(r) * hd(k) * r_k, axis=-1, keepdims=True) * hd(v)
    o = (o + bonus.reshape(B, S, C)) * g
    return o.astype(p.dtype) @ w_rwkv_out


def hierarchical_moe(h, w_group_router, b_group_router, w_expert_router, b_expert_router,
                     w_expert_gate, w_expert_up, w_expert_down):
    B, S, D = h.shape
    T = B * S
    hf = h.reshape(T, D)
    grp_prob = jax.nn.softmax((hf @ w_group_router).astype(jnp.float32) + b_group_router, axis=-1)
    grp_p, grp_idx = lax.top_k(grp_prob, 1)
    exp_logits = ((hf @ w_expert_router).astype(jnp.float32) + b_expert_router).reshape(
        T, N_EXPERT_GROUPS, EXPERTS_PER_GROUP)
    sel_logits = jnp.take_along_axis(exp_logits, grp_idx[:, :, None], axis=1)[:, 0]
    top_p, top_i = lax.top_k(jax.nn.softmax(sel_logits, axis=-1), TOP_K)
    gates = grp_p * top_p / jnp.sum(top_p, axis=-1, keepdims=True)
    expert_id = grp_idx * EXPERTS_PER_GROUP + top_i

    A = T * TOP_K
    eid = expert_id.reshape(A).astype(jnp.int32)
    tok = jnp.arange(A, dtype=jnp.int32) // TOP_K
    gate = gates.reshape(A)
    order = jnp.argsort(eid)
    se = eid[order]
    counts = jnp.bincount(eid, length=N_EXPERTS)
    starts = jnp.cumsum(counts) - counts
    pcounts = (counts + ROW_BLOCK - 1) // ROW_BLOCK * ROW_BLOCK
    pends = jnp.cumsum(pcounts)
    pstarts = pends - pcounts
    dest = pstarts[se] + jnp.arange(A, dtype=jnp.int32) - starts[se]
    n_blocks = -(-A // ROW_BLOCK) + N_EXPERTS
    n_rows = n_blocks * ROW_BLOCK
    row_tok = jnp.full((n_rows,), T, jnp.int32).at[dest].set(tok[order])
    row_gate = jnp.zeros((n_rows,), jnp.float32).at[dest].set(gate[order])
    blk_expert = jnp.minimum(jnp.searchsorted(pends, jnp.arange(n_blocks) * ROW_BLOCK, side='right'),
                             N_EXPERTS - 1)
    h_pad = jnp.concatenate([hf, jnp.zeros((1, D), hf.dtype)], axis=0)
    xin = h_pad[row_tok].reshape(n_blocks, ROW_BLOCK, D)

    def expert_block(args):
        xb, e = args
        hid = jax.nn.silu(xb @ w_expert_gate[e]) * (xb @ w_expert_up[e])
        return hid @ w_expert_down[e]

    yb = lax.map(expert_block, (xin, blk_expert)).reshape(n_rows, D)
    y = jax.ops.segment_sum(yb.astype(jnp.float32) * row_gate[:, None], row_tok, num_segments=T + 1)[:T]
    return y.reshape(B, S, D).astype(h.dtype)


def hybrid_layer(x, norm1_gain, w_in, q_norm_gain, k_norm_gain, rel_bias_table, w_att_out,
                 shift_mu, w0, w_up, a0, a_up, g_up, k_k, k_a, r_k, ln_w, ln_b, w_rwkv_out,
                 w_out, norm2_gain, w_group_router, b_group_router, w_expert_router,
                 b_expert_router, w_expert_gate, w_expert_up, w_expert_down):
    B, S, D = x.shape
    h = rms_norm(x, norm1_gain)
    p = h @ w_in
    q, k, v, p_rwkv, p_gate = jnp.split(
        p, [ATT_WIDTH, 2 * ATT_WIDTH, 3 * ATT_WIDTH, 3 * ATT_WIDTH + RWKV_SHIFT_WIDTH], axis=-1)

    heads = lambda t: t.reshape(B, S, N_ATT_GROUPS, HEADS_PER_GROUP, HEAD_DIM)
    q = rms_norm(heads(q), q_norm_gain[:, None, :])
    k = rms_norm(heads(k), k_norm_gain[:, None, :])
    v = heads(v)
    outs, lses = [], []
    for g, (win, dil) in enumerate(ATT_PATTERNS):
        o_g, l_g = dilated_window_attention(
            q[:, :, g], k[:, :, g], v[:, :, g],
            rel_bias_table[:, g * HEADS_PER_GROUP:(g + 1) * HEADS_PER_GROUP], win, dil)
        outs.append(o_g)
        lses.append(l_g)
    wts = jax.nn.softmax(jnp.stack(lses, axis=0), axis=0)
    att = jnp.einsum('gbsh,gbshe->bshe', wts, jnp.stack(outs, axis=0).astype(jnp.float32))
    y_att = att.reshape(B, S, ATT_OUT_WIDTH).astype(x.dtype) @ w_att_out

    y_rwkv = rwkv7_time_mix(p_rwkv, shift_mu, w0, w_up, a0, a_up, g_up, k_k, k_a, r_k,
                            ln_w, ln_b, w_rwkv_out)

    gates = jax.nn.sigmoid(p_gate.astype(jnp.float32)).astype(x.dtype)
    mixed = gates[..., :D] * y_att + gates[..., D:] * y_rwkv
    x = x + mixed @ w_out

    x = x + hierarchical_moe(rms_norm(x, norm2_gain), w_group_router, b_group_router,
                             w_expert_router, b_expert_router, w_expert_gate, w_expert_up,
                             w_expert_down)
    return x


def setup_inputs(seed: int = 0) -> dict:
    key = jax.random.key(seed)
    ks = jax.random.split(key, 28)
    f32 = jnp.float32
    L, D = DEPTH, D_MODEL
    nrm = lambda k, shape, scale: scale * jax.random.normal(k, shape, f32)
    return {
        'x': nrm(ks[0], (BATCH, SEQ, D), 1.0),
        'norm1_gain': 1.0 + nrm(ks[1], (L, D), 0.05),
        'w_in': nrm(ks[2], (L, D, IN_WIDTH), D ** -0.5),
        'q_norm_gain': 1.0 + nrm(ks[3], (L, N_ATT_GROUPS, HEAD_DIM), 0.05),
        'k_norm_gain': 1.0 + nrm(ks[4], (L, N_ATT_GROUPS, HEAD_DIM), 0.05),
        'rel_bias_table': nrm(ks[5], (REL_BUCKETS, ATT_HEADS), 0.5),
        'w_att_out': nrm(ks[6], (L, ATT_OUT_WIDTH, D), ATT_OUT_WIDTH ** -0.5),
        'rwkv_shift_mu': jax.random.uniform(ks[7], (L, RWKV_SHIFT_WIDTH), f32),
        'rwkv_w0': jax.random.uniform(ks[8], (L, RWKV_WIDTH), f32, -6.0, 1.0),
        'rwkv_w_up': nrm(ks[9], (L, DECAY_LORA, RWKV_WIDTH), 0.5 * DECAY_LORA ** -0.5),
        'rwkv_a0': nrm(ks[10], (L, RWKV_WIDTH), 0.1),
        'rwkv_a_up': nrm(ks[11], (L, ICLR_LORA, RWKV_WIDTH), ICLR_LORA ** -0.5),
        'rwkv_g_up': nrm(ks[12], (L, GATE_LORA, RWKV_WIDTH), GATE_LORA ** -0.5),
        'rwkv_k_k': 0.85 + nrm(ks[13], (L, RWKV_WIDTH), 0.05),
        'rwkv_k_a': 1.0 + nrm(ks[14], (L, RWKV_WIDTH), 0.05),
        'rwkv_r_k': nrm(ks[15], (L, RWKV_HEADS, RWKV_HEAD_DIM), 0.1),
        'rwkv_ln_w': 1.0 + nrm(ks[16], (L, RWKV_WIDTH), 0.05),
        'rwkv_ln_b': nrm(ks[17], (L, RWKV_WIDTH), 0.01),
        'w_rwkv_out': nrm(ks[18], (L, RWKV_WIDTH, D), RWKV_WIDTH ** -0.5),
        'w_out': nrm(ks[19], (L, D, D), D ** -0.5),
        'norm2_gain': 1.0 + nrm(ks[20], (L, D), 0.05),
        'w_group_router': nrm(ks[21], (L, D, N_EXPERT_GROUPS), D ** -0.5),
        'b_group_router': nrm(ks[22], (L, N_EXPERT_GROUPS), 0.01),
        'w_expert_router': nrm(ks[23], (L, D, N_EXPERTS), D ** -0.5),
        'b_expert_router': nrm(ks[24], (L, N_EXPERTS), 0.01),
        'w_expert_gate': nrm(ks[25], (L, N_EXPERTS, D, D_EXPERT), D ** -0.5),
        'w_expert_up': nrm(ks[26], (L, N_EXPERTS, D, D_EXPERT), D ** -0.5),
        'w_expert_down': nrm(ks[27], (L, N_EXPERTS, D_EXPERT, D), D_EXPERT ** -0.5),
    }


def reference(x, norm1_gain, w_in, q_norm_gain, k_norm_gain, rel_bias_table, w_att_out,
              rwkv_shift_mu, rwkv_w0, rwkv_w_up, rwkv_a0, rwkv_a_up, rwkv_g_up, rwkv_k_k,
              rwkv_k_a, rwkv_r_k, rwkv_ln_w, rwkv_ln_b, w_rwkv_out, w_out, norm2_gain,
              w_group_router, b_group_router, w_expert_router, b_expert_router,
              w_expert_gate, w_expert_up, w_expert_down):
    for l in range(DEPTH):
        x = hybrid_layer(x, norm1_gain[l], w_in[l], q_norm_gain[l], k_norm_gain[l], rel_bias_table,
                         w_att_out[l], rwkv_shift_mu[l], rwkv_w0[l], rwkv_w_up[l], rwkv_a0[l],
                         rwkv_a_up[l], rwkv_g_up[l], rwkv_k_k[l], rwkv_k_a[l], rwkv_r_k[l],
                         rwkv_ln_w[l], rwkv_ln_b[l], w_rwkv_out[l], w_out[l], norm2_gain[l],
                         w_group_router[l], b_group_router[l], w_expert_router[l],
                         b_expert_router[l], w_expert_gate[l], w_expert_up[l], w_expert_down[l])
    return x
```

```python
import math
from contextlib import ExitStack

import numpy as np
import concourse.bass as bass
import concourse.mybir as mybir
from concourse.bass_utils import run_bass_kernel_spmd

F32 = mybir.dt.float32
BF16 = mybir.dt.bfloat16
AF = mybir.ActivationFunctionType
ALU = mybir.AluOpType
AX = mybir.AxisListType

N_CORES = 8
T = 2048
D = 1024
NSEQ = 2
IN_W = 6176
DILS = (1, 4, 16)
C_RW = 512
OFF_RW = 2304
OFF_GATE = 2304 + 1824
NEXP = 32
DEXP = 512


class Buf:
    __slots__ = ("name", "last_w", "readers", "dma_key", "dma_cnt", "excl")

    def __init__(self, name, excl=False):
        self.name = name
        self.excl = excl
        self.last_w = None
        self.readers = {}
        self.dma_key = None
        self.dma_cnt = 0


class Prog:
    ENGS = ("pe", "dve", "act", "pool", "sp")

    def __init__(self, nc):
        self.nc = nc
        self.eng = {"pe": nc.tensor, "dve": nc.vector, "act": nc.scalar, "pool": nc.gpsimd, "sp": nc.sync}
        self.stack = ExitStack()
        self.sem = {}
        for e in self.ENGS:
            self.sem[e] = self.stack.enter_context(nc.semaphore("s_" + e))
        self.cnt = {e: 0 for e in self.ENGS}
        self.seen = {e: {} for e in self.ENGS}
        self.dma_keys = []
        self.out_events = []
        import os
        self.kcut = int(os.environ.get("KCUT", "0"))
        self.nops = 0
        self.last_rows = (0, 128)
        self.trace_lines = [] if os.environ.get("KTRACE") else None

    def _deps(self, eng, reads, writes):
        waits = {}
        seen = self.seen[eng]

        def need(k, v):
            if seen.get(k, 0) < v and waits.get(k, 0) < v:
                waits[k] = v
        for b in reads:
            if b.last_w is not None:
                need(*b.last_w)
        for b in writes:
            if b.last_w is not None:
                need(*b.last_w)
            for k, v in b.readers.items():
                need(k, v)
        E = self.eng[eng]
        for k, v in waits.items():
            seen[k] = v
            E.wait_ge(self.sem[k], v)

    def _commit(self, ev, reads, writes):
        k, v = ev
        for b in reads:
            if b.readers.get(k, 0) < v:
                b.readers[k] = v
        for b in writes:
            b.last_w = ev
            b.readers = {}

    def op(self, eng, fn, reads=(), writes=(), rows=(0, 128)):
        self.nops += 1
        if self.trace_lines is not None:
            import sys as _s
            self.trace_lines.append((self.nops, _s._getframe(1).f_lineno))
        if self.kcut and self.nops > self.kcut:
            return
        if any(b.excl for b in reads):
            writes = list(writes) + [b for b in reads if b.excl]
            reads = [b for b in reads if not b.excl]
        self._deps(eng, reads, writes)
        if eng == "pe":
            if rows != self.last_rows and self.seen["pe"].get("pe", 0) < self.cnt["pe"]:
                self.eng["pe"].wait_ge(self.sem["pe"], self.cnt["pe"])
                self.seen["pe"]["pe"] = self.cnt["pe"]
            self.last_rows = rows
        inst = fn(self.eng[eng])
        self.cnt[eng] += 1
        inst.then_inc(self.sem[eng], 1)
        self._commit((eng, self.cnt[eng]), reads, writes)

    def dma(self, q, fn, sbuf, reads=(), writes=(), is_out=False):
        self.nops += 1
        if self.trace_lines is not None:
            import sys as _s
            self.trace_lines.append((self.nops, _s._getframe(1).f_lineno))
        if self.kcut and self.nops > self.kcut:
            return
        self._deps(q, reads, writes)
        if sbuf.dma_key is None:
            sbuf.dma_key = ("dma", len(self.dma_keys))
            self.sem[sbuf.dma_key] = self.stack.enter_context(self.nc.semaphore("s_dma%d" % len(self.dma_keys)))
            self.dma_keys.append(sbuf)
        inst = fn(self.eng[q])
        sbuf.dma_cnt += 1
        inst.then_inc(self.sem[sbuf.dma_key], 16)
        ev = (sbuf.dma_key, 16 * sbuf.dma_cnt)
        self._commit(ev, reads, writes)
        if is_out:
            self.out_events.append(ev)

    def barrier(self):
        for e in self.ENGS:
            E = self.eng[e]
            seen = self.seen[e]
            for f in self.ENGS:
                if self.cnt[f] > seen.get(f, 0):
                    E.wait_ge(self.sem[f], self.cnt[f])
                    seen[f] = self.cnt[f]
            for b in self.dma_keys:
                v = 16 * b.dma_cnt
                if v > seen.get(b.dma_key, 0):
                    E.wait_ge(self.sem[b.dma_key], v)
                    seen[b.dma_key] = v

    def finish(self):
        self.barrier()
        self.stack.close()


def PB(name):
    return Buf(name, excl=True)


def _bucket_table():
    dist = np.arange(0, 2049)
    d = np.maximum(dist.astype(np.float32), np.float32(1.0))
    large = 16 + (np.log(d / np.float32(16.0)) / np.float32(math.log(2048 / 16)) * np.float32(16)).astype(np.int32)
    large = np.minimum(large, 31)
    return np.where(dist < 16, dist, large)


def build_program(stage=99, dbg=False):
    nc = bass.Bass("TRN2", target_bir_lowering=False)
    dram = lambda n, s, dt=F32, kind="ExternalInput": nc.dram_tensor(n, list(s), dt, kind=kind).ap()
    x_d = dram("x", [NSEQ, T, D])
    y_d = dram("y", [NSEQ, T, D], kind="ExternalOutput")
    w_in = dram("w_in", [D, IN_W])
    g1col_d = dram("g1col", [128, 8])
    g2col_d = dram("g2col", [128, 8])
    qkgc_d = dram("qkgc", [128, 6])
    biasg_d = dram("biasg", [128, 12 * 256])
    amask_d = dram("amask", [128, 256])
    ident_d = dram("ident", [128, 128])
    rwcol_d = dram("rwcol", [128, 43])
    lora_d = dram("lora", [128, 512])
    gup_d = dram("gup", [160, 512])
    rwmask_d = dram("rwmask", [128, 768 + 1024])
    wout_d = dram("w_out", [D, D])
    wao_d = dram("w_att_out", [256, D])
    wro_d = dram("w_rwkv_out", [512, D])
    wrt_d = dram("w_router", [D, 36])
    rbias_d = dram("b_router", [36])
    weg_d = dram("w_expert_gate", [NEXP, D, DEXP])
    weu_d = dram("w_expert_up", [NEXP, D, DEXP])
    wed_d = dram("w_expert_down", [NEXP, DEXP, D])
    dbg_d = {}
    if dbg:
        dbg_d["hT"] = dram("dbg_hT", [128, 8 * T], kind="ExternalOutput")
        dbg_d["att"] = dram("dbg_att", [64, 4 * T], kind="ExternalOutput")
        dbg_d["o_rw"] = dram("dbg_o_rw", [128, 4 * T], kind="ExternalOutput")
        dbg_d["gate"] = dram("dbg_gate", [128, 16 * 32], kind="ExternalOutput")

    P = Prog(nc)
    S = P.stack
    uid = [0]

    def sb(n, s, dt=F32, st=S):
        uid[0] += 1
        return st.enter_context(nc.sbuf_tensor("sb%d_%s" % (uid[0], n), list(s), dt))

    def psum(n, s, dt=F32, st=S):
        uid[0] += 1
        return st.enter_context(nc.psum_tensor("ps%d_%s" % (uid[0], n), list(s), dt))

    ident_f = sb("ident_f", [128, 128]); ident_b = sb("ident_b", [128, 128], BF16)
    g1col = sb("g1col", [128, 8]); g2col = sb("g2col", [128, 8])
    ones_b = sb("ones_b", [128, 64], BF16)
    nhalf = sb("nhalf", [128, 256], F32)
    bC = Buf("consts")
    P.dma("sp", lambda e: e.dma_start(out=ident_f[:], in_=ident_d[:, :]), bC, writes=[bC])
    P.dma("sp", lambda e: e.dma_start(out=g1col[:], in_=g1col_d[:, :]), bC, writes=[bC])
    P.dma("sp", lambda e: e.dma_start(out=g2col[:], in_=g2col_d[:, :]), bC, writes=[bC])
    P.op("dve", lambda e: e.tensor_copy(out=ident_b[:], in_=ident_f[:]), reads=[bC], writes=[bC])
    P.op("pool", lambda e: e.memset(ones_b[:], 1.0), writes=[bC])
    P.op("pool", lambda e: e.memset(nhalf[:], -0.5), writes=[bC])

    b_yd = Buf("y_dram")

    for b in range(NSEQ if stage >= 50 else 1):
        stA = ExitStack()
        hT = sb("hT", [128, 8, T], BF16, stA)
        b_hT = Buf("hT")
        attT = sb("attT", [64, 4, T], BF16, stA)
        b_attT = Buf("attT")
        with ExitStack() as st1:
            xt = [sb("xt%d" % i, [128, D], F32, st1) for i in range(3)]
            bx = [Buf("xt%d" % i) for i in range(3)]
            sq = [sb("sq%d" % i, [128, D], BF16, st1) for i in range(2)]; bsq = [Buf("sq%d" % i) for i in range(2)]
            xn = [sb("xn%d" % i, [128, D], BF16, st1) for i in range(2)]
            bxn = [Buf("xn%d" % i) for i in range(2)]
            ss = sb("ss", [128, 16], F32, st1); bss = [Buf("ss%d" % i) for i in range(16)]
            ptr = [psum("ptr%d" % i, [128, 8, 128], BF16, st1) for i in range(2)]
            bptr = [PB("ptr%d" % i) for i in range(2)]

            def front1(tt):
                i3 = tt % 3; i2 = tt % 2
                P.dma("sp", lambda e: e.dma_start(out=xt[i3][:], in_=x_d[b, tt * 128:(tt + 1) * 128, :]), bx[i3], writes=[bx[i3]])
                P.op("act", lambda e: e.activation(out=sq[i2][:], in_=xt[i3][:], func=AF.Square), reads=[bx[i3]], writes=[bsq[i2]])
                P.op("dve", lambda e: e.tensor_reduce(out=ss[:, tt:tt + 1], in_=sq[i2][:], axis=AX.X, op=ALU.add), reads=[bsq[i2]], writes=[bss[tt]])
                P.op("dve", lambda e: e.tensor_scalar(out=ss[:, tt:tt + 1], in0=ss[:, tt:tt + 1], scalar1=1.0 / D, scalar2=1e-6, op0=ALU.mult, op1=ALU.add), reads=[bss[tt]], writes=[bss[tt]])
                P.op("act", lambda e: e.activation(out=ss[:, tt:tt + 1], in_=ss[:, tt:tt + 1], func=AF.Ln), reads=[bss[tt]], writes=[bss[tt]])
                P.op("act", lambda e: e.activation(out=ss[:, tt:tt + 1], in_=ss[:, tt:tt + 1], func=AF.Exp, scale=-0.5), reads=[bss[tt]], writes=[bss[tt]])
                P.op("act", lambda e: e.activation(out=xn[i2][:], in_=xt[i3][:], func=AF.Copy, scale=ss[:, tt:tt + 1]), reads=[bx[i3], bss[tt]], writes=[bxn[i2]])

            def back1(tt):
                i2 = tt % 2

                def tr(e):
                    for kc in range(8):
                        r = e.transpose(out=ptr[i2][:, kc, :], in_=xn[i2][:, kc * 128:(kc + 1) * 128], identity=ident_b[:])
                    return r
                P.op("pe", tr, reads=[bxn[i2], bC], writes=[bptr[i2]])
                P.op("dve", lambda e: e.tensor_tensor(
                    out=hT[:, :, tt * 128:(tt + 1) * 128], in0=ptr[i2][:],
                    in1=g1col[:].unsqueeze(2).to_broadcast([128, 8, 128]), op=ALU.mult), reads=[bptr[i2], bC], writes=[b_hT])
            front1(0)
            for tt in range(16):
                if tt + 1 < 16:
                    front1(tt + 1)
                back1(tt)
        P.barrier()
        if dbg and b == 0:
            P.dma("pool", lambda e: e.dma_start(out=dbg_d["hT"][:, :], in_=hT[:].rearrange("p a t -> p (a t)")), b_hT, reads=[b_hT], is_out=True)
        if stage < 2:
            stA.close()
            continue

        with ExitStack() as st2:
            Etab = sb("Etab", [128, 12 * 256], F32, st2)
            amask = sb("amask", [128, 256], F32, st2)
            bC2 = Buf("consts2")
            P.dma("sp", lambda e: e.dma_start(out=Etab[:], in_=biasg_d[:, :]), bC2, writes=[bC2])
            P.dma("sp", lambda e: e.dma_start(out=amask[:], in_=amask_d[:, :]), bC2, writes=[bC2])
            P.op("act", lambda e: e.activation(out=Etab[:], in_=Etab[:], func=AF.Exp), reads=[bC2], writes=[bC2])
            P.op("dve", lambda e: e.tensor_tensor(
                out=Etab[:].rearrange("p (h c) -> p h c", h=12), in0=Etab[:].rearrange("p (h c) -> p h c", h=12),
                in1=amask[:].unsqueeze(1).to_broadcast([128, 12, 256]), op=ALU.mult), reads=[bC2], writes=[bC2])
            wg = [sb("wg%d" % i, [128, 8, 768], BF16, st2) for i in range(2)]
            bwg = [Buf("wg%d" % i) for i in range(2)]
            qkT = sb("qkT", [128, 4, T], BF16, st2)
            vg = sb("vg", [128, 16, 256], BF16, st2)
            bqk = [Buf("qk%d" % i) for i in range(16)]
            bv = [Buf("v%d" % i) for i in range(16)]
            accn = sb("accn", [64, 4, T], F32, st2); accd = sb("accd", [64, 4, T], F32, st2)
            bacc = [Buf("acc%d" % i) for i in range(4)]
            sq2 = [sb("sq2_%d" % i, [128, 512], BF16, st2) for i in range(2)]; bsq2 = [Buf("sq2_%d" % i) for i in range(2)]
            ssq = [sb("ssq_%d" % i, [128, 8], F32, st2) for i in range(2)]; bssq = [Buf("ssq_%d" % i) for i in range(2)]
            qkn = [sb("qkn_%d" % i, [128, 512], BF16, st2) for i in range(2)]; bqkn = [Buf("qkn_%d" % i) for i in range(2)]
            qkgc = sb("qkgc", [128, 6], F32, st2)
            P.dma("sp", lambda e: e.dma_start(out=qkgc[:], in_=qkgc_d[:, :]), bC2, writes=[bC2])
            pe_f = [[sb("pe_f%d_%d" % (i, r), [128, 512], BF16, st2) for r in range(2)] for i in range(2)]; bpe_f = [[Buf("pe_f%d_%d" % (i, r)) for r in range(2)] for i in range(2)]
            pb_ = [[sb("pb_%d_%d" % (i, r), [128, 512], BF16, st2) for r in range(2)] for i in range(2)]; bpb = [[Buf("pb%d_%d" % (i, r)) for r in range(2)] for i in range(2)]
            ps_qk2 = [psum("ps_qk%d" % i, [128, 512], F32, st2) for i in range(2)]; bps_qk2 = [PB("ps_qk%d" % i) for i in range(2)]
            ps_vt2 = [psum("ps_vt%d" % i, [128, 512], F32, st2) for i in range(2)]; bps_vt2 = [PB("ps_vt%d" % i) for i in range(2)]
            ps_s = [psum("ps_s%d" % i, [128, 512], F32, st2) for i in range(2)]; bps_s = [PB("ps_s%d" % i) for i in range(2)]
            ps_o = [psum("ps_o%d" % i, [128, 512], F32, st2) for i in range(2)]; bps_o = [PB("ps_o%d" % i) for i in range(2)]
            cnt_h = 0
            for g in range(3):
                d = DILS[g]
                L = T // d
                nblk = L // 128
                wgi = g % 2
                for j in range(3):
                    c0 = j * 768 + g * 256
                    P.dma("pool", lambda e, j=j, c0=c0, wgi=wgi: e.dma_start(
                        out=wg[wgi][:, :, j * 256:(j + 1) * 256],
                        in_=w_in[:, c0:c0 + 256].rearrange("(kc p) n -> p kc n", p=128)), bwg[wgi], writes=[bwg[wgi]])
                def tile_geom(st_):
                    r = st_ // nblk
                    n = st_ % nblk
                    tok0 = n * 128 * d + r
                    return n, slice(tok0, tok0 + 127 * d + 1, d)

                def front(st_):
                    n, tsl = tile_geom(st_)
                    fi = st_ % 2
                    pqk = ps_qk2[fi]; bpqk = bps_qk2[fi]; pvt = ps_vt2[fi]; bpvt = bps_vt2[fi]
                    pvt_b = pvt[:].bitcast(BF16)

                    def mmqkv(e):
                        for kc in range(8):
                            e.matmul(pqk[:, :], lhsT=hT[:, kc, tsl], rhs=wg[wgi][:, kc, 0:512], start=(kc == 0), stop=(kc == 7))
                        for kc in range(8):
                            rr = e.matmul(pvt[:, 0:256], lhsT=hT[:, kc, tsl], rhs=wg[wgi][:, kc, 512:768], start=(kc == 0), stop=(kc == 7))
                        return rr
                    P.op("pe", mmqkv, reads=[b_hT, bwg[wgi]], writes=[bpqk, bpvt])
                    P.op("act", lambda e: e.activation(out=sq2[fi][:], in_=pqk[:, :], func=AF.Square), reads=[bpqk], writes=[bsq2[fi]])
                    P.op("dve", lambda e: e.tensor_reduce(out=ssq[fi][:], in_=sq2[fi][:].rearrange("p (h c) -> p h c", h=8), axis=AX.X, op=ALU.add), reads=[bsq2[fi]], writes=[bssq[fi]])
                    P.op("dve", lambda e: e.tensor_scalar(out=ssq[fi][:], in0=ssq[fi][:], scalar1=1.0 / 64, scalar2=1e-6, op0=ALU.mult, op1=ALU.add), reads=[bssq[fi]], writes=[bssq[fi]])
                    P.op("act", lambda e: e.activation(out=ssq[fi][:], in_=ssq[fi][:], func=AF.Ln), reads=[bssq[fi]], writes=[bssq[fi]])
                    P.op("act", lambda e: e.activation(out=ssq[fi][:], in_=ssq[fi][:], func=AF.Exp, scale=-0.5), reads=[bssq[fi]], writes=[bssq[fi]])
                    P.op("dve", lambda e: e.tensor_tensor(out=qkn[fi][:].rearrange("p (h c) -> p h c", h=8), in0=pqk[:, :].rearrange("p (h c) -> p h c", h=8),
                                                          in1=ssq[fi][:].unsqueeze(2).to_broadcast([128, 8, 64]), op=ALU.mult), reads=[bpqk, bssq[fi]], writes=[bqkn[fi]])
                    P.op("act", lambda e: e.activation(out=vg[:, st_, :], in_=pvt[:, 0:256], func=AF.Copy), reads=[bpvt], writes=[bv[st_]])

                def front2(st_):
                    fi = st_ % 2
                    pvt = ps_vt2[fi]; bpvt = bps_vt2[fi]
                    pvt_b = pvt[:].bitcast(BF16)

                    def trqk(e):
                        for j in range(4):
                            rr = e.transpose(out=pvt_b[:, 512 + j * 128:512 + (j + 1) * 128], in_=qkn[fi][:, j * 128:(j + 1) * 128], identity=ident_b[:])
                        return rr
                    P.op("pe", trqk, reads=[bqkn[fi], bC], writes=[bpvt])
                    P.op("act", lambda e: e.activation(out=qkT[:, 0:2, st_ * 128:(st_ + 1) * 128], in_=pvt_b[:, 512:768].rearrange("p (a t) -> p a t", a=2), func=AF.Copy, scale=qkgc[:, 2 * g:2 * g + 1]), reads=[bpvt, bC2], writes=[bqk[st_]])
                    P.op("act", lambda e: e.activation(out=qkT[:, 2:4, st_ * 128:(st_ + 1) * 128], in_=pvt_b[:, 768:1024].rearrange("p (a t) -> p a t", a=2), func=AF.Copy, scale=qkgc[:, 2 * g + 1:2 * g + 2]), reads=[bpvt, bC2], writes=[bqk[st_]])

                def back(st_):
                    n, tsl = tile_geom(st_)
                    has_prev = n > 0
                    c_lo = 0 if has_prev else 128
                    ci = st_ % 2
                    for r_ in range(2):
                        pb0 = r_ * 64

                        def mms(e):
                            for hl in range(2):
                                pair = hl
                                q_ap = qkT[pb0:pb0 + 64, pair, st_ * 128:(st_ + 1) * 128]
                                if has_prev:
                                    e.matmul(ps_s[r_][:, hl * 256:hl * 256 + 128], lhsT=qkT[pb0:pb0 + 64, 2 + pair, (st_ - 1) * 128:st_ * 128], rhs=q_ap, start=True, stop=True)
                                rr = e.matmul(ps_s[r_][:, hl * 256 + 128:hl * 256 + 256], lhsT=qkT[pb0:pb0 + 64, 2 + pair, st_ * 128:(st_ + 1) * 128], rhs=q_ap, start=True, stop=True)
                            return rr
                        rd = [bqk[st_]] + ([bqk[st_ - 1]] if has_prev else [])
                        P.op("pe", mms, reads=rd, writes=[bps_s[r_]], rows=(pb0, 64))
                    for r_ in range(2):
                        pf3 = pe_f[ci][r_][:].rearrange("p (h c) -> p h c", h=2)
                        pb3 = pb_[ci][r_][:].rearrange("p (h c) -> p h c", h=2)
                        ps3 = ps_s[r_][:, :].rearrange("p (h c) -> p h c", h=2)
                        E3 = Etab[:].rearrange("p (h c) -> p h c", h=12)[:, g * 4 + r_:g * 4 + r_ + 3:2, :]
                        P.op("act", lambda e: e.activation(out=pf3[:, :, c_lo:256], in_=ps3[:, :, c_lo:256], func=AF.Exp, scale=0.125), reads=[bps_s[r_]], writes=[bpe_f[ci][r_]])
                        P.op("dve" if r_ == 0 else "pool", lambda e: e.tensor_tensor(out=pb3[:, :, c_lo:256], in0=pf3[:, :, c_lo:256], in1=E3[:, :, c_lo:256], op=ALU.mult), reads=[bpe_f[ci][r_], bC2], writes=[bpb[ci][r_]])
                    for k_ in range(2):
                        def mmo(e):
                            for hl in range(2):
                                hh = 2 * k_ + hl
                                r_ = hh % 2
                                hsl = hh // 2
                                pcur = pb_[ci][r_][:, hsl * 256 + 128:hsl * 256 + 256]
                                pprev = pb_[ci][r_][:, hsl * 256:hsl * 256 + 128]
                                o_n = ps_o[k_][0:64, hl * 256:hl * 256 + 128]
                                o_d = ps_o[k_][0:64, hl * 256 + 128:hl * 256 + 256]
                                if has_prev:
                                    e.matmul(o_n, lhsT=vg[:, st_ - 1, hh * 64:(hh + 1) * 64], rhs=pprev, start=True, stop=False)
                                e.matmul(o_n, lhsT=vg[:, st_, hh * 64:(hh + 1) * 64], rhs=pcur, start=(not has_prev), stop=True)
                                if has_prev:
                                    e.matmul(o_d, lhsT=ones_b[:, :], rhs=pprev, start=True, stop=False)
                                rr = e.matmul(o_d, lhsT=ones_b[:, :], rhs=pcur, start=(not has_prev), stop=True)
                            return rr
                        rd = [bpb[ci][0], bpb[ci][1], bv[st_], bC] + ([bv[st_ - 1]] if has_prev else [])
                        P.op("pe", mmo, reads=rd, writes=[bps_o[k_]])
                        po3 = ps_o[k_][0:64, :].rearrange("p (h c) -> p h c", h=2)
                        an = accn[:, 2 * k_:2 * k_ + 2, tsl]
                        ad = accd[:, 2 * k_:2 * k_ + 2, tsl]
                        if g == 0:
                            P.op("act", lambda e: e.activation(out=an, in_=po3[:, :, 0:128], func=AF.Copy), reads=[bps_o[k_]], writes=[bacc[k_]])
                            P.op("dve", lambda e: e.tensor_copy(out=ad, in_=po3[:, :, 128:256]), reads=[bps_o[k_]], writes=[bacc[k_]])
                        else:
                            P.op("dve", lambda e: e.tensor_tensor(out=an, in0=po3[:, :, 0:128], in1=an, op=ALU.add), reads=[bps_o[k_], bacc[k_]], writes=[bacc[k_]])
                            P.op("dve", lambda e: e.tensor_tensor(out=ad, in0=po3[:, :, 128:256], in1=ad, op=ALU.add), reads=[bps_o[k_], bacc[k_]], writes=[bacc[k_]])

                front(0)
                front2(0)
                for st_ in range(16):
                    if st_ + 1 < 16:
                        front(st_ + 1)
                    back(st_)
                    if st_ + 1 < 16:
                        front2(st_ + 1)
            for hh in range(4):
                P.op("act", lambda e, hh=hh: e.activation(out=accd[:, hh, :], in_=accd[:, hh, :], func=AF.Ln), reads=[bacc[hh // 2]], writes=[bacc[hh // 2]])
                P.op("act", lambda e, hh=hh: e.activation(out=accd[:, hh, :], in_=accd[:, hh, :], func=AF.Exp, scale=-1.0), reads=[bacc[hh // 2]], writes=[bacc[hh // 2]])
                P.op("dve" if hh % 2 == 0 else "pool", lambda e, hh=hh: e.tensor_tensor(out=attT[:, hh, :], in0=accn[:, hh, :], in1=accd[:, hh, :], op=ALU.mult), reads=[bacc[hh // 2]], writes=[b_attT])
        P.barrier()
        if dbg and b == 0:
            with ExitStack() as std:
                tmp = sb("dbgtmp", [64, 4 * T], F32, std)
                bt = Buf("dbgtmp")
                P.op("dve", lambda e: e.tensor_copy(out=tmp[:], in_=attT[:].rearrange("p a t -> p (a t)")), reads=[b_attT], writes=[bt])
                P.dma("sp", lambda e: e.dma_start(out=dbg_d["att"][:, :], in_=tmp[:]), bt, reads=[bt], is_out=True)
                P.barrier()
        if stage < 3:
            stA.close()
            continue

        o_fin = sb("o_fin", [128, 4, T], BF16, stA)
        b_ofin = Buf("o_fin")
        TB = 256
        NTB = T // TB
        LAM = math.exp(-0.5)
        with ExitStack() as st3:
            w_rw = sb("w_rw", [128, 8, 1824], BF16, st3); b_wrwc = [Buf("w_rw%d" % i) for i in range(4)]
            lora = sb("lora", [128, 512], BF16, st3)
            guA = sb("guA", [128, 512], BF16, st3); guB = sb("guB", [32, 512], BF16, st3)
            b_w3 = Buf("w3small")
            rwcol = sb("rwcol", [128, 43], F32, st3); omm = sb("omm", [128, 15], F32, st3); omka = sb("omka", [128, 4], F32, st3)
            mask3 = sb("mask3", [128, 3, 128], F32, st3); mask_su = sb("mask_su", [128, 128], F32, st3); mask_sl = sb("mask_sl", [128, 128], F32, st3)
            bones = sb("bones", [128, 128], F32, st3); resetm = sb("resetm", [128, 4 * TB], F32, st3)
            b_c3 = Buf("c3")
            for (ci_, c0_, c1_) in ((3, 1536, 1824), (0, 0, 512), (1, 512, 1024), (2, 1024, 1536)):
                P.dma("pool", lambda e: e.dma_start(out=w_rw[:, :, c0_:c1_], in_=w_in[:, OFF_RW + c0_:OFF_RW + c1_].rearrange("(kc p) n -> p kc n", p=128)), b_wrwc[ci_], writes=[b_wrwc[ci_]])
            P.dma("pool", lambda e: e.dma_start(out=lora[:], in_=lora_d[:, :]), b_w3, writes=[b_w3])
            P.dma("pool", lambda e: e.dma_start(out=guA[:], in_=gup_d[0:128, :]), b_w3, writes=[b_w3])
            P.dma("pool", lambda e: e.dma_start(out=guB[:], in_=gup_d[128:160, :]), b_w3, writes=[b_w3])
            P.dma("sp", lambda e: e.dma_start(out=rwcol[:], in_=rwcol_d[:, :]), b_c3, writes=[b_c3])
            P.dma("sp", lambda e: e.dma_start(out=mask_su[:], in_=rwmask_d[:, 0:128]), b_c3, writes=[b_c3])
            P.dma("sp", lambda e: e.dma_start(out=mask3[:].rearrange("p a t -> p (a t)"), in_=rwmask_d[:, 128:512]), b_c3, writes=[b_c3])
            P.dma("sp", lambda e: e.dma_start(out=mask_sl[:], in_=rwmask_d[:, 512:640]), b_c3, writes=[b_c3])
            P.dma("sp", lambda e: e.dma_start(out=bones[:], in_=rwmask_d[:, 640:768]), b_c3, writes=[b_c3])
            P.dma("sp", lambda e: e.dma_start(out=resetm[:], in_=rwmask_d[:, 768:768 + 4 * TB]), b_c3, writes=[b_c3])
            P.op("dve", lambda e: e.tensor_scalar(out=omm[:], in0=rwcol[:, 0:15], scalar1=-1.0, scalar2=1.0, op0=ALU.mult, op1=ALU.add), reads=[b_c3], writes=[b_c3])
            P.op("dve", lambda e: e.tensor_scalar(out=omka[:], in0=rwcol[:, 27:31], scalar1=-1.0, scalar2=1.0, op0=ALU.mult, op1=ALU.add), reads=[b_c3], writes=[b_c3])
            hcol = sb("hcol", [128, 8], F32, st3)
            P.op("dve", lambda e: e.tensor_scalar(out=hcol[:], in0=rwcol[:, 15:23], scalar1=0.5, scalar2=None, op0=ALU.mult), reads=[b_c3], writes=[b_c3])

            def t2(n, w=TB, dt=F32, p=128):
                return sb(n, [p, w], dt, st3), Buf(n)
            raw = [t2("raw%d" % i, TB + 1) for i in range(2)]
            tmpm = [t2("tmpm%d" % i) for i in range(2)]
            car, b_car = t2("car", 15)
            waT, b_waT = t2("waT"); gaT, b_gaT = t2("gaT"); gbT, b_gbT = t2("gbT")
            twa, b_twa = t2("twa", TB, BF16); sgA, b_sgA = t2("sgA", TB, BF16); sgB, b_sgB = t2("sgB", TB, BF16, 32)
            def w4(n, dt=F32):
                return sb(n, [128, 4, TB], dt, st3), Buf(n)
            rT4, b_rT4 = w4("rT4"); kT4, b_kT4 = w4("kT4"); vT4, b_vT4 = w4("vT4")
            sgz4, b_sgz4 = w4("sgz4"); aT4, b_aT4 = w4("aT4"); kk4, b_kk4 = w4("kk4"); sqk4, b_sqk4 = w4("sqk4")
            kkn4, b_kkn4 = w4("kkn4"); k24, b_k24 = w4("k24"); bT4, b_bT4 = w4("bT4"); ta4, b_ta4 = w4("ta4")
            e14, b_e14 = w4("e14"); bonus4, b_bonus4 = w4("bonus4"); gT4, b_gT4 = w4("gT4")
            xc4 = kk4; b_xc4 = b_kk4; sqo4 = sqk4; b_sqo4 = b_sqk4
            raw4 = [sb("raw4_%d" % i, [128, 4, TB + 1], F32, st3) for i in range(2)]; b_raw4 = [Buf("raw4_%d" % i) for i in range(2)]
            RK4 = sb("RK4", [128, 4, 2, 2, 128], BF16, st3); b_RK4 = Buf("RK4")
            BT4, b_BT4 = w4("BT4", BF16); KT4, b_KT4 = w4("KT4", BF16); vTb4, b_vTb4 = w4("vTb4", BF16)
            TMs = [[sb("TM%d" % u, [128, 4, 128], BF16, st3) for u in range(2)]]; b_TMs = [[Buf("TM%d" % u) for u in range(2)]]
            XXs = [[sb("XX%d" % i, [128, 2, 128], BF16, st3) for i in range(4)]]; b_XXs = [[Buf("XX%d" % i) for i in range(4)]]
            UUs = [[sb("UU%d" % i, [128, 2, 128], BF16, st3) for i in range(4)]]; b_UUs = [[Buf("UU%d" % i) for i in range(4)]]
            M3s = [[sb("M3%d" % i, [128, 3, 128], BF16, st3) for i in range(4)]]; b_M3s = [[Buf("M3%d" % i) for i in range(4)]]
            RKs = [None]; b_RKs = [b_RK4]; e1s = [None]; b_e1s = [b_e14]; bonusTs = [None]; b_bonuss = [b_bonus4]; gTs = [None]; b_gTs = [b_gT4]
            chains = [(u, hd) for u in range(2) for hd in range(2)]
            Z2 = [sb("Z2%d" % i, [128, 64], BF16, st3) for i in range(4)]; b_Z2 = [Buf("Z2%d" % i) for i in range(4)]
            nW = [sb("nW%d" % i, [128, 128], BF16, st3) for i in range(4)]; b_nW = [Buf("nW%d" % i) for i in range(4)]
            RpT, b_RpT = t2("RpT")
            PhiT = sb("PhiT", [128, 4, 64], F32, st3); b_Phi = [Buf("Phi%d" % i) for i in range(8)]
            ST = [[sb("ST%d_%d" % (c, i), [128, 64], F32, st3) for i in range(2)] for c in range(4)]
            b_ST = [[[Buf("ST%d_%d_%d" % (c, i, hd)) for hd in range(2)] for i in range(2)] for c in range(4)]
            o_sb4 = sb("o_sb4", [128, 4, TB], F32, st3); b_osb4 = Buf("o_sb4")

            pr = [psum("pr%d" % i, [128, 512], F32, st3) for i in range(2)]; b_pr = [PB("pr%d" % i) for i in range(2)]
            pt = psum("pt", [128, 512], F32, st3); b_pt = PB("pt")
            pt_b = pt[:].bitcast(BF16)
            px = psum("px", [128, 512], F32, st3); b_px = PB("px")
            pm = psum("pm", [128, 512], F32, st3); b_pmh = [PB("pm_h%d" % hd) for hd in range(2)]
            pc = [psum("pc%d" % i, [128, 512], F32, st3) for i in range(2)]; b_pc = [PB("pc0"), PB("pc1"), b_px, b_pt]
            po = psum("po", [128, 512], F32, st3); b_po = [PB("po_h%d" % hd) for hd in range(2)]
            cbanks = [pc[0], pc[1], px, pt]
            pcs = lambda ch: cbanks[ch][:, 0:256]
            prc = [0]

            def next_pr():
                i = prc[0] % 2
                prc[0] += 1
                return pr[i], b_pr[i]

            P.op("pool", lambda e: e.memset(car[:], 0.0), writes=[b_car])
            for c in range(4):
                P.op("pool", lambda e, c=c: e.memset(ST[c][0][:], 0.0), writes=[b_ST[c][0][0], b_ST[c][0][1]])
            rawc = [0]

            def proj_shift(tb, colbase, ncols, mu_i, out_t, out_b):
                ps_, bps_ = next_pr()
                i = rawc[0] % 2
                rawc[0] += 1
                rw_, brw_ = raw[i]
                tm_, btm_ = tmpm[i]
                n = ncols

                def mm(e):
                    for kc in range(8):
                        rr = e.matmul(ps_[0:n, 0:TB], lhsT=w_rw[:, kc, colbase:colbase + n], rhs=hT[:, kc, tb * TB:(tb + 1) * TB], start=(kc == 0), stop=(kc == 7))
                    return rr
                P.op("pe", mm, reads=[b_wrwc[min(colbase // 512, 3)], b_hT], writes=[bps_])
                P.op("act", lambda e: e.activation(out=rw_[0:n, 1:TB + 1], in_=ps_[0:n, 0:TB], func=AF.Copy), reads=[bps_], writes=[brw_])
                P.op("dve", lambda e: e.tensor_copy(out=rw_[0:n, 0:1], in_=car[0:n, mu_i:mu_i + 1]), reads=[b_car], writes=[brw_])
                P.op("pool", lambda e: e.tensor_copy(out=car[0:n, mu_i:mu_i + 1], in_=rw_[0:n, TB:TB + 1]), reads=[brw_], writes=[b_car])
                P.op("act", lambda e: e.activation(out=tm_[0:n, :], in_=rw_[0:n, 0:TB], func=AF.Copy, scale=rwcol[0:n, mu_i:mu_i + 1]), reads=[brw_, b_c3], writes=[btm_])
                P.op("dve", lambda e: e.scalar_tensor_tensor(out=out_t[0:n, :], in0=rw_[0:n, 1:TB + 1], scalar=omm[0:n, mu_i:mu_i + 1], in1=tm_[0:n, :], op0=ALU.mult, op1=ALU.add), reads=[brw_, btm_, b_c3], writes=[out_b])

            def bc4(col0):
                return lambda t_: t_[:, col0:col0 + 4].unsqueeze(2).to_broadcast([128, 4, TB])
            fl = lambda t_: t_[:].rearrange("p c t -> p (c t)")
            rawk = [0]

            def stageAW(tb):
                proj_shift(tb, 1536, 128, 12, waT, b_waT)
                proj_shift(tb, 1664, 128, 13, gaT, b_gaT)
                proj_shift(tb, 1792, 32, 14, gbT, b_gbT)
                P.op("act", lambda e: e.activation(out=twa[0:64, :], in_=waT[0:64, :], func=AF.Tanh), reads=[b_waT], writes=[b_twa])
                P.op("act", lambda e: e.activation(out=twa[64:128, :], in_=waT[64:128, :], func=AF.Copy), reads=[b_waT], writes=[b_twa])
                P.op("act", lambda e: e.activation(out=gaT[:], in_=gaT[:], func=AF.Tanh, scale=0.5), reads=[b_gaT], writes=[b_gaT])
                P.op("act", lambda e: e.activation(out=gbT[0:32, :], in_=gbT[0:32, :], func=AF.Tanh, scale=0.5), reads=[b_gbT], writes=[b_gbT])
                P.op("dve", lambda e: e.tensor_scalar(out=sgA[:], in0=gaT[:], scalar1=0.5, scalar2=0.5, op0=ALU.mult, op1=ALU.add), reads=[b_gaT], writes=[b_sgA])
                P.op("dve", lambda e: e.tensor_scalar(out=sgB[:], in0=gbT[0:32, :], scalar1=0.5, scalar2=0.5, op0=ALU.mult, op1=ALU.add), reads=[b_gbT], writes=[b_sgB])
                for (base, ci, out4, bout) in ((0, 0, rT4, b_rT4), (512, 4, kT4, b_kT4), (1024, 8, vT4, b_vT4)):
                    rw4 = raw4[rawk[0] % 2]; brw4 = b_raw4[rawk[0] % 2]
                    rawk[0] += 1
                    for c in range(4):
                        ps_, bps_ = next_pr()

                        def mm(e):
                            for kc in range(8):
                                rr = e.matmul(ps_[:, 0:TB], lhsT=w_rw[:, kc, base + c * 128:base + (c + 1) * 128], rhs=hT[:, kc, tb * TB:(tb + 1) * TB], start=(kc == 0), stop=(kc == 7))
                            return rr
                        P.op("pe", mm, reads=[b_wrwc[base // 512], b_hT], writes=[bps_])
                        P.op("act", lambda e: e.activation(out=rw4[:, c, 1:TB + 1], in_=ps_[:, 0:TB], func=AF.Copy), reads=[bps_], writes=[brw4])
                    P.op("dve", lambda e: e.tensor_copy(out=rw4[:, :, 0:1], in_=car[:, ci:ci + 4].unsqueeze(2)), reads=[b_car], writes=[brw4])
                    P.op("pool", lambda e: e.tensor_copy(out=car[:, ci:ci + 4].unsqueeze(2), in_=rw4[:, :, TB:TB + 1]), reads=[brw4], writes=[b_car])
                    P.op("pool", lambda e: e.tensor_tensor(out=ta4[:], in0=rw4[:, :, 0:TB], in1=bc4(ci)(rwcol), op=ALU.mult), reads=[brw4, b_c3], writes=[b_ta4])
                    P.op("dve", lambda e: e.tensor_tensor(out=out4[:], in0=rw4[:, :, 1:TB + 1], in1=bc4(ci)(omm), op=ALU.mult), reads=[brw4, b_c3], writes=[bout])
                    P.op("dve", lambda e: e.tensor_tensor(out=out4[:], in0=out4[:], in1=ta4[:], op=ALU.add), reads=[b_ta4], writes=[bout])
                for c in range(4):
                    pz, bpz = next_pr()
                    P.op("pe", lambda e: e.matmul(pz[:, 0:TB], lhsT=lora[0:64, c * 128:(c + 1) * 128], rhs=twa[0:64, :], start=True, stop=True), reads=[b_w3, b_twa], writes=[bpz], rows=(0, 64))
                    P.op("act", lambda e: e.activation(out=sgz4[:, c, :], in_=pz[:, 0:TB], func=AF.Tanh, bias=hcol[:, c:c + 1], scale=0.5), reads=[bpz, b_c3], writes=[b_sgz4])
                P.op("dve", lambda e: e.tensor_scalar(out=fl(sgz4), in0=fl(sgz4), scalar1=0.5, scalar2=0.5, op0=ALU.mult, op1=ALU.add), reads=[b_sgz4], writes=[b_sgz4])
                for c in range(4):
                    pa, bpa = next_pr()
                    P.op("pe", lambda e: e.matmul(pa[:, 0:TB], lhsT=lora[64:128, c * 128:(c + 1) * 128], rhs=twa[64:128, :], start=True, stop=True), reads=[b_w3, b_twa], writes=[bpa], rows=(64, 64))
                    P.op("act", lambda e: e.activation(out=aT4[:, c, :], in_=pa[:, 0:TB], func=AF.Tanh, bias=hcol[:, 4 + c:5 + c], scale=0.5), reads=[bpa, b_c3], writes=[b_aT4])
                P.op("pool", lambda e: e.tensor_scalar(out=fl(aT4), in0=fl(aT4), scalar1=0.5, scalar2=0.5, op0=ALU.mult, op1=ALU.add), reads=[b_aT4], writes=[b_aT4])
                for c in range(4):
                    pg, bpg = next_pr()

                    def mmg(e):
                        e.matmul(pg[:, 0:TB], lhsT=guA[:, c * 128:(c + 1) * 128], rhs=sgA[:], start=True, stop=False)
                        return e.matmul(pg[:, 0:TB], lhsT=guB[0:32, c * 128:(c + 1) * 128], rhs=sgB[0:32, :], start=False, stop=True)
                    P.op("pe", mmg, reads=[b_w3, b_sgA, b_sgB], writes=[bpg])
                    P.op("act", lambda e: e.activation(out=gT4[:, c, :], in_=pg[:, 0:TB], func=AF.Copy), reads=[bpg], writes=[b_gT4])
                for c in range(4):
                    P.op("act", lambda e: e.activation(out=kk4[:, c, :], in_=kT4[:, c, :], func=AF.Copy, scale=rwcol[:, 23 + c:24 + c]), reads=[b_kT4, b_c3], writes=[b_kk4])
                    P.op("act", lambda e: e.activation(out=sqk4[:, c, :], in_=kT4[:, c, :], func=AF.Square, scale=rwcol[:, 23 + c:24 + c]), reads=[b_kT4, b_c3], writes=[b_sqk4])
                for hf in range(2):
                    pn, bpn = next_pr()
                    P.op("pe", lambda e: e.matmul(pn[:, :], lhsT=bones[:], rhs=fl(sqk4)[:, hf * 512:(hf + 1) * 512], start=True, stop=True), reads=[b_c3, b_sqk4], writes=[bpn])
                    P.op("dve", lambda e: e.tensor_scalar(out=fl(sqk4)[:, hf * 512:(hf + 1) * 512], in0=pn[:, :], scalar1=1e-18, scalar2=None, op0=ALU.max), reads=[bpn], writes=[b_sqk4])
                P.op("act", lambda e: e.activation(out=fl(sqk4), in_=fl(sqk4), func=AF.Ln), reads=[b_sqk4], writes=[b_sqk4])
                P.op("act", lambda e: e.activation(out=fl(sqk4), in_=fl(sqk4), func=AF.Exp, scale=-0.5), reads=[b_sqk4], writes=[b_sqk4])
                P.op("dve", lambda e: e.tensor_tensor(out=kkn4[:], in0=kk4[:], in1=sqk4[:], op=ALU.mult), reads=[b_kk4, b_sqk4], writes=[b_kkn4])
                for c in range(4):
                    P.op("act", lambda e: e.activation(out=kk4[:, c, :], in_=aT4[:, c, :], func=AF.Identity, scale=rwcol[:, 27 + c:28 + c], bias=omka[:, c:c + 1]), reads=[b_aT4, b_c3, b_kkn4], writes=[b_kk4])
                P.op("pool", lambda e: e.tensor_tensor(out=k24[:], in0=kT4[:], in1=kk4[:], op=ALU.mult), reads=[b_kT4, b_kk4], writes=[b_k24])
                P.op("dve", lambda e: e.tensor_tensor(out=bT4[:], in0=kkn4[:], in1=aT4[:], op=ALU.mult), reads=[b_kkn4, b_aT4], writes=[b_bT4])
                P.op("dve", lambda e: e.tensor_tensor(out=sqk4[:], in0=rT4[:], in1=bc4(31)(rwcol), op=ALU.mult), reads=[b_rT4, b_c3, b_kkn4], writes=[b_sqk4])
                P.op("dve", lambda e: e.tensor_tensor(out=sqk4[:], in0=sqk4[:], in1=k24[:], op=ALU.mult), reads=[b_k24], writes=[b_sqk4])
                for hf in range(2):
                    pbn, bpbn = next_pr()
                    P.op("pe", lambda e: e.matmul(pbn[:, :], lhsT=bones[:], rhs=fl(sqk4)[:, hf * 512:(hf + 1) * 512], start=True, stop=True), reads=[b_c3, b_sqk4], writes=[bpbn])
                    P.op("dve", lambda e: e.tensor_tensor(out=fl(bonus4)[:, hf * 512:(hf + 1) * 512], in0=pbn[:, :], in1=fl(vT4)[:, hf * 512:(hf + 1) * 512], op=ALU.mult), reads=[bpbn, b_vT4], writes=[b_bonus4])
                P.op("dve", lambda e: e.tensor_tensor_scan(out=fl(ta4), data0=resetm[:], data1=fl(sgz4), initial=0.0, op0=ALU.mult, op1=ALU.add), reads=[b_c3, b_sgz4], writes=[b_ta4])
                P.op("act", lambda e: e.activation(out=fl(e14), in_=fl(ta4), func=AF.Exp, scale=-LAM), reads=[b_ta4], writes=[b_e14])
                P.op("pool", lambda e: e.tensor_tensor(out=sgz4[:], in0=ta4[:], in1=sgz4[:], op=ALU.subtract), reads=[b_ta4], writes=[b_sgz4])
                P.op("act", lambda e: e.activation(out=fl(sgz4), in_=fl(sgz4), func=AF.Exp, scale=-LAM), reads=[b_sgz4], writes=[b_sgz4])
                P.op("act", lambda e: e.activation(out=fl(ta4), in_=fl(ta4), func=AF.Exp, scale=LAM), reads=[b_sgz4, b_e14], writes=[b_ta4])
                v4 = lambda t_: t_[:].rearrange("p c (u t) -> p c u t", u=2)
                P.op("pool", lambda e: e.tensor_tensor(out=RK4[:, :, :, 0, :], in0=v4(kkn4), in1=v4(sgz4), op=ALU.mult), reads=[b_kkn4, b_sgz4], writes=[b_RK4])
                P.op("dve", lambda e: e.tensor_tensor(out=RK4[:, :, :, 1, :], in0=v4(rT4), in1=v4(e14), op=ALU.mult), reads=[b_rT4, b_e14], writes=[b_RK4])
                P.op("pool", lambda e: e.tensor_tensor(out=BT4[:], in0=bT4[:], in1=ta4[:], op=ALU.mult), reads=[b_bT4, b_ta4], writes=[b_BT4])
                P.op("pool", lambda e: e.tensor_tensor(out=KT4[:], in0=k24[:], in1=ta4[:], op=ALU.mult), reads=[b_k24, b_ta4], writes=[b_KT4])
                P.op("act", lambda e: e.activation(out=fl(vTb4), in_=fl(vT4), func=AF.Copy), reads=[b_vT4], writes=[b_vTb4])

            def stageA(tb, c, sl):
                RK = RK4[:, c]; b_RK = b_RK4
                BT = BT4[:, c, :]; KT = KT4[:, c, :]; vTb = vTb4[:, c, :]
                b_BT = b_BT4; b_KT = b_KT4; b_vTb = b_vTb4
                RKs[0] = RK; e1s[0] = e14[:, c, :]; bonusTs[0] = bonus4[:, c, :]; gTs[0] = gT4[:, c, :]
                TM = TMs[sl]; b_TM = b_TMs[sl]; XX = XXs[sl]; b_XX = b_XXs[sl]
                UU = UUs[sl]; b_UU = b_UUs[sl]; M3 = M3s[sl]; b_M3 = b_M3s[sl]
                for u in range(2):
                    usl = slice(u * 128, (u + 1) * 128)

                    def trs(e):
                        e.transpose(out=pt_b[:, 0:128], in_=RK[:, u, 0, :], identity=ident_b[:])
                        e.transpose(out=pt_b[:, 128:256], in_=BT[:, usl], identity=ident_b[:])
                        e.transpose(out=pt_b[:, 256:384], in_=KT[:, usl], identity=ident_b[:])
                        return e.transpose(out=pt_b[:, 384:512], in_=vTb[:, usl], identity=ident_b[:])
                    P.op("pe", trs, reads=[b_RK, b_BT, b_KT, b_vTb, bC], writes=[b_pt])
                    P.op("act", lambda e: e.activation(out=TM[u][:].rearrange("p a t -> p (a t)"), in_=pt_b[:, 0:512], func=AF.Copy), reads=[b_pt], writes=[b_TM[u]])
                    yield
                xbanks = [(px, b_px), (pc[0], b_pc[0]), (pc[1], b_pc[1]), (pm, b_pmh[0])]
                x3slots = [(pt[:, 256:384], b_pt), (pt[:, 384:512], b_pt), (po[:, 0:128], b_po[0]), (po[:, 128:256], b_po[0])]
                for ch in (0, 2, 1, 3):
                    u, hd = chains[ch]
                    pb0 = hd * 64
                    usl = slice(u * 128, (u + 1) * 128)
                    xb, bxb = xbanks[ch]
                    x3, bx3 = x3slots[ch]
                    wr = [bxb, bx3] + ([b_pmh[1]] if ch == 3 else []) + ([b_po[1]] if ch >= 2 else [])

                    def cross(e):
                        rk = RK[pb0:pb0 + 64, u, :, :].rearrange("p a t -> p (a t)")
                        e.matmul(xb[:, 0:256], lhsT=BT[pb0:pb0 + 64, usl], rhs=rk, start=True, stop=True)
                        e.matmul(xb[:, 256:512], lhsT=KT[pb0:pb0 + 64, usl], rhs=rk, start=True, stop=True)
                        return e.matmul(x3, lhsT=RK[pb0:pb0 + 64, u, 0, :], rhs=BT[pb0:pb0 + 64, usl], start=True, stop=True)
                    P.op("pe", cross, reads=[b_RK, b_BT, b_KT], writes=wr, rows=(pb0, 64))
                    P.op("dve", lambda e: e.tensor_tensor(out=XX[ch][:, 0, :], in0=xb[:, 0:128], in1=mask_su[:], op=ALU.mult), reads=[bxb, b_c3] + ([b_pmh[1]] if ch == 3 else []), writes=[b_XX[ch]])
                    P.op("dve", lambda e: e.tensor_tensor(out=M3[ch][:].rearrange("p a t -> p (a t)"), in0=xb[:, 128:512], in1=mask3[:].rearrange("p a t -> p (a t)"), op=ALU.mult), reads=[bxb, b_c3] + ([b_pmh[1]] if ch == 3 else []), writes=[b_M3[ch]])
                    P.op("dve", lambda e: e.tensor_tensor(out=XX[ch][:, 1, :], in0=x3, in1=mask_sl[:], op=ALU.mult), reads=[bx3] + ([b_po[1]] if ch >= 2 else []) + [b_c3], writes=[b_XX[ch]])
                    P.op("pool", lambda e: e.tensor_tensor(out=UU[ch][:], in0=ident_f[:].unsqueeze(1).to_broadcast([128, 2, 128]), in1=XX[ch][:], op=ALU.subtract), reads=[bC, b_XX[ch]], writes=[b_UU[ch]])
                    yield

            def stageB(tb, c, sl):
                RK = RKs[sl]; b_RK = b_RKs[sl]; TM = TMs[sl]; b_TM = b_TMs[sl]; XX = XXs[sl]; b_XX = b_XXs[sl]
                UU = UUs[sl]; b_UU = b_UUs[sl]; M3 = M3s[sl]; b_M3 = b_M3s[sl]
                e1 = e1s[sl]; b_e1 = b_e1s[sl]; bonusT = bonusTs[sl]; b_bonus = b_bonuss[sl]; gT = gTs[sl]; b_gT = b_gTs[sl]
                for k in range(1, 6):
                    last = (k == 5)
                    for ch in range(4):
                        def sqr(e):
                            rr = e.matmul(pcs(ch)[:, 0:128], lhsT=XX[ch][:, 1, :], rhs=XX[ch][:, 0, :], start=True, stop=True)
                            if not last:
                                rr = e.matmul(pcs(ch)[:, 128:256], lhsT=XX[ch][:, 0, :], rhs=XX[ch][:, 1, :], start=True, stop=True)
                            return rr
                        P.op("pe", sqr, reads=[b_XX[ch]], writes=[b_pc[ch]])
                        if last:
                            P.op("act", lambda e: e.activation(out=XX[ch][:, 0, :], in_=pcs(ch)[:, 0:128], func=AF.Copy), reads=[b_pc[ch]], writes=[b_XX[ch]])
                        else:
                            P.op("act", lambda e: e.activation(out=XX[ch][:].rearrange("p a t -> p (a t)"), in_=pcs(ch)[:, 0:256], func=AF.Copy), reads=[b_pc[ch]], writes=[b_XX[ch]])
                        yield
                    for ch in range(4):
                        def app(e):
                            rr = e.matmul(pcs(ch)[:, 0:128], lhsT=UU[ch][:, 1, :], rhs=XX[ch][:, 0, :], start=True, stop=True)
                            if not last:
                                rr = e.matmul(pcs(ch)[:, 128:256], lhsT=XX[ch][:, 0, :], rhs=UU[ch][:, 1, :], start=True, stop=True)
                            return rr
                        P.op("pe", app, reads=[b_XX[ch], b_UU[ch]], writes=[b_pc[ch]])
                        if last:
                            P.op("dve", lambda e: e.tensor_tensor(out=UU[ch][:, 0, :], in0=pcs(ch)[:, 0:128], in1=UU[ch][:, 0, :], op=ALU.add), reads=[b_pc[ch], b_UU[ch]], writes=[b_UU[ch]])
                        else:
                            P.op("dve", lambda e: e.tensor_tensor(out=UU[ch][:].rearrange("p a t -> p (a t)"), in0=pcs(ch)[:, 0:256], in1=UU[ch][:].rearrange("p a t -> p (a t)"), op=ALU.add), reads=[b_pc[ch], b_UU[ch]], writes=[b_UU[ch]])
                        yield
                for ch, (u, hd) in enumerate(chains):
                    pb0 = hd * 64
                    P.op("pe", lambda e: e.matmul(pcs(ch)[:, 0:64], lhsT=M3[ch][:, 1, :], rhs=TM[u][:, 3, pb0:pb0 + 64], start=True, stop=True), reads=[b_M3[ch], b_TM[u]], writes=[b_pc[ch]])
                    P.op("act", lambda e: e.activation(out=Z2[ch][:], in_=pcs(ch)[:, 0:64], func=AF.Copy), reads=[b_pc[ch]], writes=[b_Z2[ch]])
                    yield
                for ch, (u, hd) in enumerate(chains):
                    pb0 = hd * 64

                    def mmw(e):
                        e.matmul(pcs(ch)[:, 128:192], lhsT=UU[ch][:, 0, :], rhs=TM[u][:, 0, pb0:pb0 + 64], start=True, stop=True)
                        return e.matmul(pcs(ch)[:, 192:256], lhsT=UU[ch][:, 0, :], rhs=Z2[ch][:], start=True, stop=True)
                    P.op("pe", mmw, reads=[b_UU[ch], b_TM[u], b_Z2[ch]], writes=[b_pc[ch]])
                    P.op("act", lambda e: e.activation(out=nW[ch][:], in_=pcs(ch)[:, 128:256], func=AF.Copy, scale=-1.0), reads=[b_pc[ch]], writes=[b_nW[ch]])
                    yield
                for ch, (u, hd) in enumerate(chains):
                    pb0 = hd * 64
                    P.op("pe", lambda e: e.matmul(cbanks[ch][pb0:pb0 + 64, 0:128], lhsT=nW[ch][:, 0:64], rhs=M3[ch][:, 0, :], start=True, stop=True), reads=[b_nW[ch], b_M3[ch]], writes=[b_pc[ch]])
                    P.op("dve", lambda e: e.tensor_tensor(out=RpT[pb0:pb0 + 64, u * 128:(u + 1) * 128], in0=cbanks[ch][pb0:pb0 + 64, 0:128], in1=RK[pb0:pb0 + 64, u, 1, :], op=ALU.add), reads=[b_pc[ch], b_RK], writes=[b_RpT])
                    yield
                for s in (0, 2, 1, 3):
                    u = s // 2; t0 = (s % 2) * 64
                    for hd in range(2):
                        pb0 = hd * 64
                        ch = u * 2 + hd
                        P.op("pe", lambda e: e.matmul(cbanks[ch][pb0:pb0 + 64, 256 + (s % 2) * 64:256 + (s % 2) * 64 + 64], lhsT=nW[ch][t0:t0 + 64, 0:64], rhs=TM[u][t0:t0 + 64, 1, pb0:pb0 + 64], start=True, stop=True), reads=[b_nW[ch], b_TM[u]], writes=[b_pc[ch]], rows=(t0, 64))
                        P.op("dve", lambda e: e.tensor_tensor(out=PhiT[pb0:pb0 + 64, s, :], in0=cbanks[ch][pb0:pb0 + 64, 256 + (s % 2) * 64:256 + (s % 2) * 64 + 64], in1=ident_f[pb0:pb0 + 64, pb0:pb0 + 64], op=ALU.add), reads=[b_pc[ch], bC], writes=[b_Phi[s * 2 + hd]])
                        yield
                for s in range(4):
                    u = s // 2; s2 = s % 2; t0 = s2 * 64
                    gs = tb * 4 + s
                    cur = gs % 2; nxt = (gs + 1) % 2
                    for hd in range(2):
                        pb0 = hd * 64
                        ch = u * 2 + hd
                        o_ap = po[pb0:pb0 + 64, s * 64:(s + 1) * 64]

                        def mo1(e):
                            e.matmul(o_ap, lhsT=TM[u][t0:t0 + 64, 3, pb0:pb0 + 64], rhs=M3[ch][t0:t0 + 64, 2, s2 * 64:(s2 + 1) * 64], start=True, stop=False)
                            return e.matmul(o_ap, lhsT=nW[ch][t0:t0 + 64, 64:128], rhs=M3[ch][t0:t0 + 64, 0, s2 * 64:(s2 + 1) * 64], start=False, stop=False)
                        s_ap = pm[pb0:pb0 + 64, 256 + (s % 2) * 64:256 + (s % 2) * 64 + 64]

                        def ms1(e):
                            e.matmul(s_ap, lhsT=TM[u][t0:t0 + 64, 2, pb0:pb0 + 64], rhs=TM[u][t0:t0 + 64, 3, pb0:pb0 + 64], start=True, stop=False)
                            return e.matmul(s_ap, lhsT=TM[u][t0:t0 + 64, 1, pb0:pb0 + 64], rhs=nW[ch][t0:t0 + 64, 64:128], start=False, stop=False)
                        P.op("pe", mo1, reads=[b_TM[u], b_M3[ch], b_nW[ch]], writes=[b_po[hd]], rows=(t0, 64))
                        P.op("pe", ms1, reads=[b_TM[u], b_nW[ch]], writes=[b_pmh[hd]], rows=(t0, 64))
                        P.op("pe", lambda e: e.matmul(o_ap, lhsT=ST[c][cur][pb0:pb0 + 64, :], rhs=RpT[pb0:pb0 + 64, s * 64:(s + 1) * 64], start=False, stop=True),
                             reads=[b_ST[c][cur][hd], b_RpT], writes=[b_po[hd]], rows=(pb0, 64))
                        P.op("pe", lambda e: e.matmul(s_ap, lhsT=PhiT[pb0:pb0 + 64, s, :], rhs=ST[c][cur][pb0:pb0 + 64, :], start=False, stop=True),
                             reads=[b_Phi[s * 2 + hd], b_ST[c][cur][hd]], writes=[b_pmh[hd]], rows=(pb0, 64))
                        P.op("act", lambda e: e.activation(
                            out=ST[c][nxt][pb0:pb0 + 64, :], in_=pm[pb0:pb0 + 64, 256 + (s % 2) * 64:256 + (s % 2) * 64 + 64],
                            func=AF.Copy, scale=e1[pb0:pb0 + 64, s * 64 + 63:s * 64 + 64]),
                            reads=[b_pmh[hd], b_e1], writes=[b_ST[c][nxt][hd]])
                        yield
                P.op("act", lambda e: e.activation(out=o_sb4[:, c, :], in_=po[:, 0:TB], func=AF.Copy), reads=[b_po[0], b_po[1]], writes=[b_osb4])
                yield

            def stageGN(tb):
                for hf in range(2):
                    hs = slice(hf * 512, (hf + 1) * 512)
                    p1, bp1 = next_pr()
                    P.op("pe", lambda e: e.matmul(p1[:, :], lhsT=bones[:], rhs=fl(o_sb4)[:, hs], start=True, stop=True), reads=[b_c3, b_osb4], writes=[bp1])
                    P.op("dve", lambda e: e.scalar_tensor_tensor(out=fl(xc4)[:, hs], in0=p1[:, :], scalar=-1.0 / 64, in1=fl(o_sb4)[:, hs], op0=ALU.mult, op1=ALU.add), reads=[bp1, b_osb4], writes=[b_xc4])
                P.op("pool", lambda e: e.tensor_tensor(out=sqo4[:], in0=xc4[:], in1=xc4[:], op=ALU.mult), reads=[b_xc4], writes=[b_sqo4])
                for hf in range(2):
                    hs = slice(hf * 512, (hf + 1) * 512)
                    p2, bp2 = next_pr()
                    P.op("pe", lambda e: e.matmul(p2[:, :], lhsT=bones[:], rhs=fl(sqo4)[:, hs], start=True, stop=True), reads=[b_c3, b_sqo4], writes=[bp2])
                    P.op("dve", lambda e: e.tensor_scalar(out=fl(sqo4)[:, hs], in0=p2[:, :], scalar1=1.0 / 64, scalar2=64e-5, op0=ALU.mult, op1=ALU.add), reads=[bp2], writes=[b_sqo4])
                P.op("act", lambda e: e.activation(out=fl(sqo4), in_=fl(sqo4), func=AF.Ln), reads=[b_sqo4], writes=[b_sqo4])
                P.op("act", lambda e: e.activation(out=fl(sqo4), in_=fl(sqo4), func=AF.Exp, scale=-0.5), reads=[b_sqo4], writes=[b_sqo4])
                P.op("dve", lambda e: e.tensor_tensor(out=xc4[:], in0=xc4[:], in1=sqo4[:], op=ALU.mult), reads=[b_sqo4], writes=[b_xc4])
                P.op("pool", lambda e: e.tensor_tensor(out=xc4[:], in0=xc4[:], in1=bc4(35)(rwcol), op=ALU.mult), reads=[b_c3], writes=[b_xc4])
                P.op("pool", lambda e: e.tensor_tensor(out=xc4[:], in0=xc4[:], in1=bc4(39)(rwcol), op=ALU.add), reads=[b_c3], writes=[b_xc4])
                P.op("dve", lambda e: e.tensor_tensor(out=xc4[:], in0=xc4[:], in1=bonus4[:], op=ALU.add), reads=[b_bonus4], writes=[b_xc4])
                P.op("dve", lambda e: e.tensor_tensor(out=o_fin[:, :, tb * TB:(tb + 1) * TB], in0=xc4[:], in1=gT4[:], op=ALU.mult), reads=[b_xc4, b_gT4], writes=[b_ofin])

            for tb in range(NTB):
                stageAW(tb)
                for c in range(4):
                    for _ in stageA(tb, c, 0):
                        pass
                    for _ in stageB(tb, c, 0):
                        pass
                stageGN(tb)
        P.barrier()
        if dbg and b == 0:
            with ExitStack() as std:
                tmp = sb("dbgtmp2", [128, 4 * T], F32, std)
                bt = Buf("dbgtmp2")
                P.op("dve", lambda e: e.tensor_copy(out=tmp[:], in_=o_fin[:].rearrange("p a t -> p (a t)")), reads=[b_ofin], writes=[bt])
                P.dma("sp", lambda e: e.dma_start(out=dbg_d["o_rw"][:, :], in_=tmp[:]), bt, reads=[bt], is_out=True)
                P.barrier()
        if stage < 4:
            stA.close()
            continue

        with ExitStack() as st4:
            w_gt = sb("w_gt", [128, 8, 2048], BF16, st4)
            w_o = sb("w_o", [128, 8, 1024], BF16, st4)
            w_ao = sb("w_ao", [64, 4, 1024], BF16, st4)
            w_ro = sb("w_ro", [128, 4, 1024], BF16, st4)
            b_w4 = Buf("w4"); b_wo4 = Buf("wo4"); b_wg4 = [Buf("wg4_%d" % i) for i in range(4)]
            P.dma("pool", lambda e: e.dma_start(out=w_ao[:, :, :], in_=wao_d.rearrange("(h e) n -> e h n", e=64)), b_w4, writes=[b_w4])
            P.dma("pool", lambda e: e.dma_start(out=w_ro[:, :, :], in_=wro_d.rearrange("(c p) n -> p c n", p=128)), b_w4, writes=[b_w4])
            for q4 in range(4):
                for off in (0, 1024):
                    c0 = off + q4 * 256
                    P.dma("pool", lambda e: e.dma_start(out=w_gt[:, :, c0:c0 + 256], in_=w_in[:, OFF_GATE + c0:OFF_GATE + c0 + 256].rearrange("(kc p) n -> p kc n", p=128)), b_wg4[q4], writes=[b_wg4[q4]])
            P.dma("pool", lambda e: e.dma_start(out=w_o[:, :, :], in_=wout_d.rearrange("(kc p) n -> p kc n", p=128)), b_wo4, writes=[b_wo4])
            g1s = sb("g1s", [128, 512], F32, st4); b_g1s = Buf("g1s")
            g2s = sb("g2s", [128, 512], F32, st4); b_g2s = Buf("g2s")
            m1 = sb("m1", [128, 512], F32, st4); b_m1 = Buf("m1")
            m2 = sb("m2", [128, 512], F32, st4); b_m2 = Buf("m2")
            mixedT = sb("mixedT", [128, 8, 512], BF16, st4); b_mix = [Buf("mix%d" % i) for i in range(8)]
            xr = [sb("xr%d" % i, [128, D], F32, st4) for i in range(2)]; b_xr = [Buf("xr%d" % i) for i in range(2)]
            x1t = [sb("x1t%d" % i, [128, D], F32, st4) for i in range(2)]; b_x1t = [Buf("x1t%d" % i) for i in range(2)]
            p_att = psum("p_att", [128, 512], F32, st4); b_patt = PB("p_att")
            p_rw = psum("p_rw", [128, 512], F32, st4); b_prw = PB("p_rw")
            p_g1 = psum("p_g1", [128, 512], F32, st4); b_pg1 = PB("p_g1")
            p_g2 = psum("p_g2", [128, 512], F32, st4); b_pg2 = PB("p_g2")
            p_out = [psum("p_out%d" % i, [128, 512], F32, st4) for i in range(2)]; b_pout = [PB("p_out%d" % i) for i in range(2)]
            for tq in range(4):
                qsl = slice(tq * 512, (tq + 1) * 512)
                for fc in range(8):
                    fsl = slice(fc * 128, (fc + 1) * 128)

                    def mm_g(e, pg, off):
                        for kc in range(8):
                            rr = e.matmul(pg[:, :], lhsT=w_gt[:, kc, off + fc * 128:off + (fc + 1) * 128], rhs=hT[:, kc, qsl], start=(kc == 0), stop=(kc == 7))
                        return rr
                    P.op("pe", lambda e: mm_g(e, p_g1, 0), reads=[b_wg4[fc // 2], b_hT], writes=[b_pg1])
                    P.op("pe", lambda e: mm_g(e, p_g2, 1024), reads=[b_wg4[fc // 2], b_hT], writes=[b_pg2])

                    def mm_rw(e):
                        for c in range(4):
                            rr = e.matmul(p_rw[:, :], lhsT=w_ro[:, c, fsl], rhs=o_fin[:, c, qsl], start=(c == 0), stop=(c == 3))
                        return rr
                    P.op("pe", mm_rw, reads=[b_w4, b_ofin], writes=[b_prw])

                    def mm_att(e):
                        for hh in range(4):
                            rr = e.matmul(p_att[:, :], lhsT=w_ao[0:64, hh, fsl], rhs=attT[0:64, hh, qsl], start=(hh == 0), stop=(hh == 3))
                        return rr
                    P.op("pe", mm_att, reads=[b_w4, b_attT], writes=[b_patt], rows=(0, 64))
                    P.op("act", lambda e: e.activation(out=g1s[:], in_=p_g1[:, :], func=AF.Sigmoid), reads=[b_pg1], writes=[b_g1s])
                    P.op("act", lambda e: e.activation(out=g2s[:], in_=p_g2[:, :], func=AF.Sigmoid), reads=[b_pg2], writes=[b_g2s])
                    P.op("dve", lambda e: e.tensor_tensor(out=m1[:], in0=p_att[:, :], in1=g1s[:], op=ALU.mult), reads=[b_patt, b_g1s], writes=[b_m1])
                    P.op("dve", lambda e: e.tensor_tensor(out=m2[:], in0=p_rw[:, :], in1=g2s[:], op=ALU.mult), reads=[b_prw, b_g2s], writes=[b_m2])
                    P.op("pool", lambda e: e.tensor_tensor(out=mixedT[:, fc, :], in0=m1[:], in1=m2[:], op=ALU.add), reads=[b_m1, b_m2], writes=[b_mix[fc]])
                for t4 in range(4):
                    tt = tq * 4 + t4
                    i2 = tt % 2
                    P.dma("sp", lambda e: e.dma_start(out=xr[i2][:], in_=x_d[b, tt * 128:(tt + 1) * 128, :]), b_xr[i2], writes=[b_xr[i2]])
                    for half in range(2):
                        def mm_o(e):
                            for kc in range(8):
                                rr = e.matmul(p_out[half][:, :], lhsT=mixedT[:, kc, t4 * 128:(t4 + 1) * 128], rhs=w_o[:, kc, half * 512:(half + 1) * 512], start=(kc == 0), stop=(kc == 7))
                            return rr
                        P.op("pe", mm_o, reads=b_mix + [b_wo4], writes=[b_pout[half]])
                        P.op("dve", lambda e: e.tensor_tensor(out=x1t[i2][:, half * 512:(half + 1) * 512], in0=p_out[half][:, :], in1=xr[i2][:, half * 512:(half + 1) * 512], op=ALU.add),
                             reads=[b_pout[half], b_xr[i2]], writes=[b_x1t[i2]])
                    P.dma("sp", lambda e: e.dma_start(out=y_d[b, tt * 128:(tt + 1) * 128, :], in_=x1t[i2][:]), b_x1t[i2], reads=[b_x1t[i2]], is_out=True)
        stA.close()
        P.barrier()
        if stage < 5:
            continue

        with ExitStack() as st5:
            x1s = sb("x1s", [128, 16, D], F32, st5); b_x1s = [Buf("x1s%d" % i) for i in range(16)]
            h2T = sb("h2T", [128, 8, T], BF16, st5); b_h2T = Buf("h2T")
            w_rt = sb("w_rt", [128, 8, 36], F32, st5); rbias = sb("rbias", [128, 36], F32, st5); b_c5 = Buf("c5")
            P.dma("sp", lambda e: e.dma_start(out=w_rt[:, :, :], in_=wrt_d.rearrange("(kc p) n -> p kc n", p=128)), b_c5, writes=[b_c5])
            P.dma("sp", lambda e: e.dma_start(out=rbias[:], in_=rbias_d.partition_broadcast(128)), b_c5, writes=[b_c5])
            weg = [sb("weg%d" % i, [128, 8, 512], BF16, st5) for i in range(2)]
            weu = [sb("weu%d" % i, [128, 8, 512], BF16, st5) for i in range(2)]
            wed = [sb("wed%d" % i, [128, 4, 1024], BF16, st5) for i in range(2)]
            b_we = [Buf("we%d" % i) for i in range(2)]
            rl = sb("rl", [128, 16, 36], F32, st5); b_rl = Buf("rl")
            gate = sb("gate", [128, 16, 32], F32, st5); b_gate = Buf("gate")
            PSB = [psum("B%d" % i, [128, 512], F32, st5) for i in range(8)]; b_PSB = [PB("B%d" % i) for i in range(8)]

            def load_expert(ex):
                i = ex % 2
                P.dma("pool", lambda e: e.dma_start(out=weg[i][:, :, :], in_=weg_d[ex].rearrange("(kc p) n -> p kc n", p=128)), b_we[i], writes=[b_we[i]])
                P.dma("pool", lambda e: e.dma_start(out=weu[i][:, :, :], in_=weu_d[ex].rearrange("(kc p) n -> p kc n", p=128)), b_we[i], writes=[b_we[i]])
                P.dma("pool", lambda e: e.dma_start(out=wed[i][:, :, :], in_=wed_d[ex].rearrange("(j p) n -> p j n", p=128)), b_we[i], writes=[b_we[i]])
            load_expert(0)
            load_expert(1)
            with ExitStack() as st5a:
                sq5 = [sb("sq5_%d" % i, [128, D], BF16, st5a) for i in range(2)]; b_sq5 = [Buf("sq5_%d" % i) for i in range(2)]
                h2f = [sb("h2f_%d" % i, [128, D], F32, st5a) for i in range(2)]; b_h2f = [Buf("h2f_%d" % i) for i in range(2)]
                h2Tf = [sb("h2Tf_%d" % i, [128, 8, 128], F32, st5a) for i in range(2)]; b_h2Tf = [Buf("h2Tf_%d" % i) for i in range(2)]
                ss5 = sb("ss5", [128, 16], F32, st5a); b_ss5 = [Buf("ss5_%d" % i) for i in range(16)]
                for tt in range(16):
                    P.dma("sp", lambda e: e.dma_start(out=x1s[:, tt, :], in_=y_d[b, tt * 128:(tt + 1) * 128, :]), b_x1s[tt], writes=[b_x1s[tt]])

                def front5(tt):
                    i2 = tt % 2
                    P.op("act", lambda e: e.activation(out=sq5[i2][:], in_=x1s[:, tt, :], func=AF.Square), reads=[b_x1s[tt]], writes=[b_sq5[i2]])
                    P.op("dve", lambda e: e.tensor_reduce(out=ss5[:, tt:tt + 1], in_=sq5[i2][:], axis=AX.X, op=ALU.add), reads=[b_sq5[i2]], writes=[b_ss5[tt]])
                    P.op("dve", lambda e: e.tensor_scalar(out=ss5[:, tt:tt + 1], in0=ss5[:, tt:tt + 1], scalar1=1.0 / D, scalar2=1e-6, op0=ALU.mult, op1=ALU.add), reads=[b_ss5[tt]], writes=[b_ss5[tt]])
                    P.op("act", lambda e: e.activation(out=ss5[:, tt:tt + 1], in_=ss5[:, tt:tt + 1], func=AF.Sqrt), reads=[b_ss5[tt]], writes=[b_ss5[tt]])
                    P.op("dve", lambda e: e.reciprocal(out=ss5[:, tt:tt + 1], in_=ss5[:, tt:tt + 1]), reads=[b_ss5[tt]], writes=[b_ss5[tt]])
                    P.op("act", lambda e: e.activation(out=h2f[i2][:], in_=x1s[:, tt, :], func=AF.Copy, scale=ss5[:, tt:tt + 1]), reads=[b_x1s[tt], b_ss5[tt]], writes=[b_h2f[i2]])

                def back5(tt):
                    i2 = tt % 2
                    for half in range(2):
                        bk = 2 * i2 + half

                        def tr5(e):
                            for k4 in range(4):
                                kc = half * 4 + k4
                                rr = e.transpose(out=PSB[bk][:, k4 * 128:(k4 + 1) * 128], in_=h2f[i2][:, kc * 128:(kc + 1) * 128], identity=ident_f[:])
                            return rr
                        P.op("pe", tr5, reads=[b_h2f[i2], bC], writes=[b_PSB[bk]])
                        P.op("dve", lambda e: e.tensor_tensor(out=h2Tf[i2][:, half * 4:(half + 1) * 4, :], in0=PSB[bk][:, :].rearrange("p (a t) -> p a t", a=4),
                                                              in1=g2col[:, half * 4:(half + 1) * 4].unsqueeze(2).to_broadcast([128, 4, 128]), op=ALU.mult),
                             reads=[b_PSB[bk], bC], writes=[b_h2Tf[i2]])
                    P.op("pool", lambda e: e.tensor_copy(out=h2T[:, :, tt * 128:(tt + 1) * 128], in_=h2Tf[i2][:]), reads=[b_h2Tf[i2]], writes=[b_h2T])
                    rb = 4 + i2

                    def mm_r(e):
                        for kc in range(8):
                            rr = e.matmul(PSB[rb][:, 0:36], lhsT=h2Tf[i2][:, kc, :], rhs=w_rt[:, kc, :], start=(kc == 0), stop=(kc == 7))
                        return rr
                    P.op("pe", mm_r, reads=[b_h2Tf[i2], b_c5], writes=[b_PSB[rb]])
                    P.op("dve", lambda e: e.tensor_tensor(out=rl[:, tt, :], in0=PSB[rb][:, 0:36], in1=rbias[:], op=ALU.add), reads=[b_PSB[rb], b_c5], writes=[b_rl])
                front5(0)
                for tt in range(16):
                    if tt + 1 < 16:
                        front5(tt + 1)
                    back5(tt)
                def t5(n, shp):
                    return sb(n, shp, F32, st5a)
                gmax = t5("gmax", [128, 16]); gsh = t5("gsh", [128, 16, 4]); gsum = t5("gsum", [128, 16]); gone = t5("gone", [128, 16, 4])
                Em = t5("Em", [128, 16, 32]); m1_ = t5("m1_", [128, 16]); eq1 = t5("eq1", [128, 16, 32]); Em2 = t5("Em2", [128, 16, 32]); m2_ = t5("m2_", [128, 16])
                sel = t5("sel", [128, 16, 32]); esh = t5("esh", [128, 16, 32]); den = t5("den", [128, 16])
                b_r = Buf("routing")
                G = rl[:, :, 0:4]
                E4 = rl[:, :, 4:36].rearrange("p t (g k) -> p t g k", g=4)
                bc = lambda a, n: a[:].unsqueeze(2).to_broadcast([128, 16, n])
                R = lambda fn, eng="dve": P.op(eng, fn, reads=[b_rl], writes=[b_r])
                R(lambda e: e.tensor_reduce(out=gmax[:], in_=G, axis=AX.X, op=ALU.max))
                R(lambda e: e.tensor_tensor(out=gsh[:], in0=G, in1=bc(gmax, 4), op=ALU.subtract))
                R(lambda e: e.activation(out=gsh[:], in_=gsh[:], func=AF.Exp), "act")
                R(lambda e: e.tensor_reduce(out=gsum[:], in_=gsh[:], axis=AX.X, op=ALU.add))
                R(lambda e: e.reciprocal(out=gsum[:], in_=gsum[:]))
                R(lambda e: e.tensor_tensor(out=gone[:], in0=G, in1=bc(gmax, 4), op=ALU.is_equal))
                R(lambda e: e.tensor_scalar(out=gone[:], in0=gone[:], scalar1=1e4, scalar2=-1e4, op0=ALU.mult, op1=ALU.add))
                R(lambda e: e.tensor_tensor(out=Em[:].rearrange("p t (g k) -> p t g k", g=4), in0=E4, in1=gone[:].unsqueeze(3).to_broadcast([128, 16, 4, 8]), op=ALU.add))
                R(lambda e: e.tensor_reduce(out=m1_[:], in_=Em[:], axis=AX.X, op=ALU.max))
                R(lambda e: e.tensor_tensor(out=eq1[:], in0=Em[:], in1=bc(m1_, 32), op=ALU.is_equal))
                R(lambda e: e.scalar_tensor_tensor(out=Em2[:].rearrange("p t k -> p (t k)"), in0=eq1[:].rearrange("p t k -> p (t k)"), scalar=-1e4, in1=Em[:].rearrange("p t k -> p (t k)"), op0=ALU.mult, op1=ALU.add))
                R(lambda e: e.tensor_reduce(out=m2_[:], in_=Em2[:], axis=AX.X, op=ALU.max))
                R(lambda e: e.tensor_tensor(out=sel[:], in0=Em[:], in1=bc(m2_, 32), op=ALU.is_ge))
                R(lambda e: e.tensor_tensor(out=esh[:], in0=Em[:], in1=bc(m1_, 32), op=ALU.subtract))
                R(lambda e: e.activation(out=esh[:], in_=esh[:], func=AF.Exp), "act")
                R(lambda e: e.tensor_tensor(out=sel[:], in0=sel[:], in1=esh[:], op=ALU.mult))
                R(lambda e: e.tensor_reduce(out=den[:], in_=sel[:], axis=AX.X, op=ALU.add))
                R(lambda e: e.reciprocal(out=den[:], in_=den[:]))
                R(lambda e: e.tensor_tensor(out=den[:], in0=den[:], in1=gsum[:], op=ALU.mult))
                P.op("dve", lambda e: e.tensor_tensor(out=gate[:], in0=sel[:], in1=bc(den, 32), op=ALU.mult), reads=[b_r], writes=[b_gate])
                if dbg and b == 0:
                    P.dma("sp", lambda e: e.dma_start(out=dbg_d["gate"][:, :], in_=gate[:].rearrange("p t k -> p (t k)")), b_gate, reads=[b_gate], is_out=True)
            P.barrier()
            with ExitStack() as st5b:
                sgt = [sb("sgt%d" % i, [128, 512], F32, st5b) for i in range(2)]; b_sgt = [Buf("sgt%d" % i) for i in range(2)]
                hid = [sb("hid%d" % i, [128, 4, 512], BF16, st5b) for i in range(2)]; b_hid = [Buf("hid%d" % i) for i in range(2)]
                nex = NEXP if stage >= 6 else 2
                yc = 0
                def emit_gu(ex, tq):
                    wi = ex % 2
                    qsl = slice(tq * 512, (tq + 1) * 512)
                    hi = (ex * 4 + tq) % 2
                    for j in range(4):
                        jsl = slice(j * 128, (j + 1) * 128)
                        pgb = j % 2; pub = 2 + j % 2

                        def mm_gu(e):
                            for kc in range(8):
                                e.matmul(PSB[pgb][:, :], lhsT=weg[wi][:, kc, jsl], rhs=h2T[:, kc, qsl], start=(kc == 0), stop=(kc == 7))
                            for kc in range(8):
                                rr = e.matmul(PSB[pub][:, :], lhsT=weu[wi][:, kc, jsl], rhs=h2T[:, kc, qsl], start=(kc == 0), stop=(kc == 7))
                            return rr
                        P.op("pe", mm_gu, reads=[b_we[wi], b_h2T], writes=[b_PSB[pgb], b_PSB[pub]])
                        P.op("act", lambda e: e.activation(out=sgt[j % 2][:], in_=PSB[pgb][:, :], func=AF.Silu), reads=[b_PSB[pgb]], writes=[b_sgt[j % 2]])
                        P.op("dve", lambda e: e.tensor_tensor(out=hid[hi][:, j, :], in0=PSB[pub][:, :], in1=sgt[j % 2][:], op=ALU.mult), reads=[b_PSB[pub], b_sgt[j % 2]], writes=[b_hid[hi]])

                def emit_y(ex, tq):
                    wi = ex % 2
                    hi = (ex * 4 + tq) % 2
                    for t4 in range(4):
                        tt = tq * 4 + t4
                        for half in range(2):
                            yb = 4 + ycnt[0] % 4
                            ycnt[0] += 1

                            def mm_y(e):
                                for j in range(4):
                                    rr = e.matmul(PSB[yb][:, :], lhsT=hid[hi][:, j, t4 * 128:(t4 + 1) * 128], rhs=wed[wi][:, j, half * 512:(half + 1) * 512], start=(j == 0), stop=(j == 3))
                                return rr
                            P.op("pe", mm_y, reads=[b_hid[hi], b_we[wi]], writes=[b_PSB[yb]])
                            P.op("dve", lambda e: e.scalar_tensor_tensor(out=x1s[:, tt, half * 512:(half + 1) * 512], in0=PSB[yb][:, :], scalar=gate[:, tt, ex:ex + 1],
                                                                         in1=x1s[:, tt, half * 512:(half + 1) * 512], op0=ALU.mult, op1=ALU.add),
                                 reads=[b_PSB[yb], b_gate, b_x1s[tt]], writes=[b_x1s[tt]])

                ycnt = [0]
                units = [(ex, tq) for ex in range(nex) for tq in range(4)]
                emit_gu(*units[0])
                for k, (ex, tq) in enumerate(units):
                    if k + 1 < len(units):
                        emit_gu(*units[k + 1])
                    emit_y(ex, tq)
                    if tq == 3 and ex + 2 < nex:
                        load_expert(ex + 2)
                for tt in range(16):
                    P.dma("sp", lambda e: e.dma_start(out=y_d[b, tt * 128:(tt + 1) * 128, :], in_=x1s[:, tt, :]), b_x1s[tt], reads=[b_x1s[tt]], is_out=True)
        P.barrier()

    print("total ops", P.nops)
    if P.trace_lines is not None:
        build_program.trace_lines = P.trace_lines
    P.finish()
    return nc


def host_inputs(inputs):
    f = lambda a: np.ascontiguousarray(np.asarray(a, dtype=np.float32))
    com = {}
    com["w_in"] = f(inputs["w_in"][0])
    com["g1col"] = f(inputs["norm1_gain"][0].reshape(8, 128).T)
    com["g2col"] = f(inputs["norm2_gain"][0].reshape(8, 128).T)
    qg = inputs["q_norm_gain"][0]; kg = inputs["k_norm_gain"][0]
    com["qkgc"] = f(np.stack([np.tile(v, 2) for g in range(3) for v in (qg[g], kg[g])], axis=1))
    bt = _bucket_table()
    tab = np.asarray(inputs["rel_bias_table"], dtype=np.float32)
    jj = np.arange(128)[:, None]; ii = np.arange(128)[None, :]
    rel_prev = ii + 128 - jj
    rel_cur = ii - jj
    biasg = np.zeros((128, 12, 2, 128), np.float32)
    for g in range(3):
        d = DILS[g]
        bp = bt[np.clip(rel_prev, 0, 128) * d]
        bc = bt[np.clip(rel_cur, 0, 128) * d]
        for hh in range(4):
            biasg[:, g * 4 + hh, 0, :] = tab[bp, g * 4 + hh]
            biasg[:, g * 4 + hh, 1, :] = tab[bc, g * 4 + hh]
    com["biasg"] = f(biasg.reshape(128, 12 * 256))
    am = np.zeros((128, 2, 128), np.float32)
    am[:, 0, :] = (rel_prev <= 128)
    am[:, 1, :] = (rel_cur >= 0)
    com["amask"] = f(am.reshape(128, 256))
    com["ident"] = np.eye(128, dtype=np.float32)
    ch4 = lambda v: np.asarray(v, np.float32).reshape(4, 128).T
    mu = np.asarray(inputs["rwkv_shift_mu"][0], np.float32)
    gb_mu = np.zeros((128, 1), np.float32); gb_mu[:32, 0] = mu[1792:1824]
    cols = [ch4(mu[0:512]), ch4(mu[512:1024]), ch4(mu[1024:1536]), mu[1536:1664].reshape(128, 1), mu[1664:1792].reshape(128, 1), gb_mu,
            ch4(inputs["rwkv_w0"][0]), ch4(inputs["rwkv_a0"][0]), ch4(inputs["rwkv_k_k"][0]), ch4(inputs["rwkv_k_a"][0]),
            ch4(inputs["rwkv_r_k"][0].reshape(-1)), ch4(inputs["rwkv_ln_w"][0]), ch4(inputs["rwkv_ln_b"][0])]
    com["rwcol"] = f(np.concatenate(cols, axis=1))
    com["lora"] = f(np.concatenate([inputs["rwkv_w_up"][0], inputs["rwkv_a_up"][0]], axis=0))
    com["gup"] = f(inputs["rwkv_g_up"][0])
    jj = np.arange(128)[:, None]; tt = np.arange(128)[None, :]
    same = (jj // 64) == (tt // 64)
    su = (same & (jj < tt)).astype(np.float32); uu = (same & (jj <= tt)).astype(np.float32); sl = (same & (jj > tt)).astype(np.float32)
    bo = same.astype(np.float32)
    rs = np.ones((128, 1024), np.float32); rs[:, ::64] = 0.0
    com["rwmask"] = f(np.concatenate([su, uu, su, uu, sl, bo, rs], axis=1))
    com["w_out"] = f(inputs["w_out"][0])
    com["w_att_out"] = f(inputs["w_att_out"][0])
    com["w_rwkv_out"] = f(inputs["w_rwkv_out"][0])
    com["w_router"] = f(np.concatenate([inputs["w_group_router"][0], inputs["w_expert_router"][0]], axis=1))
    com["b_router"] = f(np.concatenate([inputs["b_group_router"][0], inputs["b_expert_router"][0]]))
    com["w_expert_gate"] = f(inputs["w_expert_gate"][0])
    com["w_expert_up"] = f(inputs["w_expert_up"][0])
    com["w_expert_down"] = f(inputs["w_expert_down"][0])
    return com


def kernel(**inputs):
    x = np.asarray(inputs["x"], dtype=np.float32)
    com = host_inputs(inputs)
    nc = build_program()
    in_maps = []
    for c in range(N_CORES):
        m = dict(com)
        m["x"] = np.ascontiguousarray(x[c * NSEQ:(c + 1) * NSEQ])
        in_maps.append(m)
    res = run_bass_kernel_spmd(nc, in_maps, core_ids=list(range(N_CORES)))
    return np.concatenate([r["y"] for r in res.results], axis=0)
```

```python
import math
from contextlib import ExitStack

import numpy as np
import concourse.bass as bass
import concourse.mybir as mybir
from concourse.bass_utils import run_bass_kernel_spmd

F32 = mybir.dt.float32
BF16 = mybir.dt.bfloat16
AF = mybir.ActivationFunctionType
ALU = mybir.AluOpType
AX = mybir.AxisListType

N_CORES = 8
T = 2048
D = 1024
NSEQ = 2
IN_W = 6176
DILS = (1, 4, 16)
C_RW = 512
OFF_RW = 2304
OFF_GATE = 2304 + 1824
NEXP = 32
DEXP = 512


class Buf:
    __slots__ = ("name", "last_w", "readers", "dma_key", "dma_cnt", "excl")

    def __init__(self, name, excl=False):
        self.name = name
        self.excl = excl
        self.last_w = None
        self.readers = {}
        self.dma_key = None
        self.dma_cnt = 0


class Prog:
    ENGS = ("pe", "dve", "act", "pool", "sp")

    def __init__(self, nc):
        self.nc = nc
        self.eng = {"pe": nc.tensor, "dve": nc.vector, "act": nc.scalar, "pool": nc.gpsimd, "sp": nc.sync}
        self.stack = ExitStack()
        self.sem = {}
        for e in self.ENGS:
            self.sem[e] = self.stack.enter_context(nc.semaphore("s_" + e))
        self.cnt = {e: 0 for e in self.ENGS}
        self.seen = {e: {} for e in self.ENGS}
        self.dma_keys = []
        self.out_events = []
        import os
        self.kcut = int(os.environ.get("KCUT", "0"))
        self.nops = 0
        self.last_rows = (0, 128)
        self.trace_lines = [] if os.environ.get("KTRACE") else None

    def _deps(self, eng, reads, writes):
        waits = {}
        seen = self.seen[eng]

        def need(k, v):
            if seen.get(k, 0) < v and waits.get(k, 0) < v:
                waits[k] = v
        for b in reads:
            if b.last_w is not None:
                need(*b.last_w)
        for b in writes:
            if b.last_w is not None:
                need(*b.last_w)
            for k, v in b.readers.items():
                need(k, v)
        E = self.eng[eng]
        for k, v in waits.items():
            seen[k] = v
            E.wait_ge(self.sem[k], v)

    def _commit(self, ev, reads, writes):
        k, v = ev
        for b in reads:
            if b.readers.get(k, 0) < v:
                b.readers[k] = v
        for b in writes:
            b.last_w = ev
            b.readers = {}

    def op(self, eng, fn, reads=(), writes=(), rows=(0, 128)):
        self.nops += 1
        if self.trace_lines is not None:
            import sys as _s
            self.trace_lines.append((self.nops, _s._getframe(1).f_lineno))
        if self.kcut and self.nops > self.kcut:
            return
        if any(b.excl for b in reads):
            writes = list(writes) + [b for b in reads if b.excl]
            reads = [b for b in reads if not b.excl]
        self._deps(eng, reads, writes)
        if eng == "pe":
            if rows != self.last_rows and self.seen["pe"].get("pe", 0) < self.cnt["pe"]:
                self.eng["pe"].wait_ge(self.sem["pe"], self.cnt["pe"])
                self.seen["pe"]["pe"] = self.cnt["pe"]
            self.last_rows = rows
        inst = fn(self.eng[eng])
        self.cnt[eng] += 1
        inst.then_inc(self.sem[eng], 1)
        self._commit((eng, self.cnt[eng]), reads, writes)

    def dma(self, q, fn, sbuf, reads=(), writes=(), is_out=False):
        self.nops += 1
        if self.trace_lines is not None:
            import sys as _s
            self.trace_lines.append((self.nops, _s._getframe(1).f_lineno))
        if self.kcut and self.nops > self.kcut:
            return
        self._deps(q, reads, writes)
        if sbuf.dma_key is None:
            sbuf.dma_key = ("dma", len(self.dma_keys))
            self.sem[sbuf.dma_key] = self.stack.enter_context(self.nc.semaphore("s_dma%d" % len(self.dma_keys)))
            self.dma_keys.append(sbuf)
        inst = fn(self.eng[q])
        sbuf.dma_cnt += 1
        inst.then_inc(self.sem[sbuf.dma_key], 16)
        ev = (sbuf.dma_key, 16 * sbuf.dma_cnt)
        self._commit(ev, reads, writes)
        if is_out:
            self.out_events.append(ev)

    def barrier(self):
        for e in self.ENGS:
            E = self.eng[e]
            seen = self.seen[e]
            for f in self.ENGS:
                if self.cnt[f] > seen.get(f, 0):
                    E.wait_ge(self.sem[f], self.cnt[f])
                    seen[f] = self.cnt[f]
            for b in self.dma_keys:
                v = 16 * b.dma_cnt
                if v > seen.get(b.dma_key, 0):
                    E.wait_ge(self.sem[b.dma_key], v)
                    seen[b.dma_key] = v

    def finish(self):
        self.barrier()
        self.stack.close()


def PB(name):
    return Buf(name, excl=True)


def _bucket_table():
    dist = np.arange(0, 2049)
    d = np.maximum(dist.astype(np.float32), np.float32(1.0))
    large = 16 + (np.log(d / np.float32(16.0)) / np.float32(math.log(2048 / 16)) * np.float32(16)).astype(np.int32)
    large = np.minimum(large, 31)
    return np.where(dist < 16, dist, large)


def build_program(stage=99, dbg=False):
    nc = bass.Bass("TRN2", target_bir_lowering=False)
    dram = lambda n, s, dt=F32, kind="ExternalInput": nc.dram_tensor(n, list(s), dt, kind=kind).ap()
    x_d = dram("x", [NSEQ, T, D])
    y_d = dram("y", [NSEQ, T, D], kind="ExternalOutput")
    w_in = dram("w_in", [D, IN_W])
    g1col_d = dram("g1col", [128, 8])
    g2col_d = dram("g2col", [128, 8])
    qkgc_d = dram("qkgc", [128, 6])
    biasg_d = dram("biasg", [128, 12 * 256])
    amask_d = dram("amask", [128, 256])
    ident_d = dram("ident", [128, 128])
    rwcol_d = dram("rwcol", [128, 43])
    lora_d = dram("lora", [128, 512])
    gup_d = dram("gup", [160, 512])
    rwmask_d = dram("rwmask", [128, 768 + 1024])
    wout_d = dram("w_out", [D, D])
    wao_d = dram("w_att_out", [256, D])
    wro_d = dram("w_rwkv_out", [512, D])
    wrt_d = dram("w_router", [D, 36])
    rbias_d = dram("b_router", [36])
    weg_d = dram("w_expert_gate", [NEXP, D, DEXP])
    weu_d = dram("w_expert_up", [NEXP, D, DEXP])
    wed_d = dram("w_expert_down", [NEXP, DEXP, D])
    dbg_d = {}
    if dbg:
        dbg_d["hT"] = dram("dbg_hT", [128, 8 * T], kind="ExternalOutput")
        dbg_d["att"] = dram("dbg_att", [64, 4 * T], kind="ExternalOutput")
        dbg_d["o_rw"] = dram("dbg_o_rw", [128, 4 * T], kind="ExternalOutput")
        dbg_d["gate"] = dram("dbg_gate", [128, 16 * 32], kind="ExternalOutput")

    P = Prog(nc)
    S = P.stack
    uid = [0]

    def sb(n, s, dt=F32, st=S):
        uid[0] += 1
        return st.enter_context(nc.sbuf_tensor("sb%d_%s" % (uid[0], n), list(s), dt))

    def psum(n, s, dt=F32, st=S):
        uid[0] += 1
        return st.enter_context(nc.psum_tensor("ps%d_%s" % (uid[0], n), list(s), dt))

    ident_f = sb("ident_f", [128, 128]); ident_b = sb("ident_b", [128, 128], BF16)
    g1col = sb("g1col", [128, 8]); g2col = sb("g2col", [128, 8])
    ones_b = sb("ones_b", [128, 64], BF16)
    nhalf = sb("nhalf", [128, 256], F32)
    bC = Buf("consts")
    P.dma("sp", lambda e: e.dma_start(out=ident_f[:], in_=ident_d[:, :]), bC, writes=[bC])
    P.dma("sp", lambda e: e.dma_start(out=g1col[:], in_=g1col_d[:, :]), bC, writes=[bC])
    P.dma("sp", lambda e: e.dma_start(out=g2col[:], in_=g2col_d[:, :]), bC, writes=[bC])
    P.op("dve", lambda e: e.tensor_copy(out=ident_b[:], in_=ident_f[:]), reads=[bC], writes=[bC])
    P.op("pool", lambda e: e.memset(ones_b[:], 1.0), writes=[bC])
    P.op("pool", lambda e: e.memset(nhalf[:], -0.5), writes=[bC])

    b_yd = Buf("y_dram")

    for b in range(NSEQ if stage >= 50 else 1):
        stA = ExitStack()
        hT = sb("hT", [128, 8, T], BF16, stA)
        b_hT = Buf("hT")
        attT = sb("attT", [64, 4, T], BF16, stA)
        b_attT = Buf("attT")
        with ExitStack() as st1:
            xt = [sb("xt%d" % i, [128, D], F32, st1) for i in range(3)]
            bx = [Buf("xt%d" % i) for i in range(3)]
            sq = [sb("sq%d" % i, [128, D], BF16, st1) for i in range(2)]; bsq = [Buf("sq%d" % i) for i in range(2)]
            xn = [sb("xn%d" % i, [128, D], BF16, st1) for i in range(2)]
            bxn = [Buf("xn%d" % i) for i in range(2)]
            ss = sb("ss", [128, 16], F32, st1); bss = [Buf("ss%d" % i) for i in range(16)]
            ptr = [psum("ptr%d" % i, [128, 8, 128], BF16, st1) for i in range(2)]
            bptr = [PB("ptr%d" % i) for i in range(2)]

            def front1(tt):
                i3 = tt % 3; i2 = tt % 2
                P.dma("sp", lambda e: e.dma_start(out=xt[i3][:], in_=x_d[b, tt * 128:(tt + 1) * 128, :]), bx[i3], writes=[bx[i3]])
                P.op("act", lambda e: e.activation(out=sq[i2][:], in_=xt[i3][:], func=AF.Square), reads=[bx[i3]], writes=[bsq[i2]])
                P.op("dve", lambda e: e.tensor_reduce(out=ss[:, tt:tt + 1], in_=sq[i2][:], axis=AX.X, op=ALU.add), reads=[bsq[i2]], writes=[bss[tt]])
                P.op("dve", lambda e: e.tensor_scalar(out=ss[:, tt:tt + 1], in0=ss[:, tt:tt + 1], scalar1=1.0 / D, scalar2=1e-6, op0=ALU.mult, op1=ALU.add), reads=[bss[tt]], writes=[bss[tt]])
                P.op("act", lambda e: e.activation(out=ss[:, tt:tt + 1], in_=ss[:, tt:tt + 1], func=AF.Ln), reads=[bss[tt]], writes=[bss[tt]])
                P.op("act", lambda e: e.activation(out=ss[:, tt:tt + 1], in_=ss[:, tt:tt + 1], func=AF.Exp, scale=-0.5), reads=[bss[tt]], writes=[bss[tt]])
                P.op("act", lambda e: e.activation(out=xn[i2][:], in_=xt[i3][:], func=AF.Copy, scale=ss[:, tt:tt + 1]), reads=[bx[i3], bss[tt]], writes=[bxn[i2]])

            def back1(tt):
                i2 = tt % 2

                def tr(e):
                    for kc in range(8):
                        r = e.transpose(out=ptr[i2][:, kc, :], in_=xn[i2][:, kc * 128:(kc + 1) * 128], identity=ident_b[:])
                    return r
                P.op("pe", tr, reads=[bxn[i2], bC], writes=[bptr[i2]])
                P.op("dve", lambda e: e.tensor_tensor(
                    out=hT[:, :, tt * 128:(tt + 1) * 128], in0=ptr[i2][:],
                    in1=g1col[:].unsqueeze(2).to_broadcast([128, 8, 128]), op=ALU.mult), reads=[bptr[i2], bC], writes=[b_hT])
            front1(0)
            for tt in range(16):
                if tt + 1 < 16:
                    front1(tt + 1)
                back1(tt)
        P.barrier()
        if dbg and b == 0:
            P.dma("pool", lambda e: e.dma_start(out=dbg_d["hT"][:, :], in_=hT[:].rearrange("p a t -> p (a t)")), b_hT, reads=[b_hT], is_out=True)
        if stage < 2:
            stA.close()
            continue

        with ExitStack() as st2:
            Etab = sb("Etab", [128, 12 * 256], F32, st2)
            amask = sb("amask", [128, 256], F32, st2)
            bC2 = Buf("consts2")
            P.dma("sp", lambda e: e.dma_start(out=Etab[:], in_=biasg_d[:, :]), bC2, writes=[bC2])
            P.dma("sp", lambda e: e.dma_start(out=amask[:], in_=amask_d[:, :]), bC2, writes=[bC2])
            P.op("act", lambda e: e.activation(out=Etab[:], in_=Etab[:], func=AF.Exp), reads=[bC2], writes=[bC2])
            P.op("dve", lambda e: e.tensor_tensor(
                out=Etab[:].rearrange("p (h c) -> p h c", h=12), in0=Etab[:].rearrange("p (h c) -> p h c", h=12),
                in1=amask[:].unsqueeze(1).to_broadcast([128, 12, 256]), op=ALU.mult), reads=[bC2], writes=[bC2])
            wg = [sb("wg%d" % i, [128, 8, 768], BF16, st2) for i in range(2)]
            bwg = [Buf("wg%d" % i) for i in range(2)]
            qkT = sb("qkT", [128, 4, T], BF16, st2)
            vg = sb("vg", [128, 16, 256], BF16, st2)
            bqk = [Buf("qk%d" % i) for i in range(16)]
            bv = [Buf("v%d" % i) for i in range(16)]
            accn = sb("accn", [64, 4, T], F32, st2); accd = sb("accd", [64, 4, T], F32, st2)
            bacc = [Buf("acc%d" % i) for i in range(4)]
            sq2 = [sb("sq2_%d" % i, [128, 512], BF16, st2) for i in range(2)]; bsq2 = [Buf("sq2_%d" % i) for i in range(2)]
            ssq = [sb("ssq_%d" % i, [128, 8], F32, st2) for i in range(2)]; bssq = [Buf("ssq_%d" % i) for i in range(2)]
            qkn = [sb("qkn_%d" % i, [128, 512], BF16, st2) for i in range(2)]; bqkn = [Buf("qkn_%d" % i) for i in range(2)]
            qkgc = sb("qkgc", [128, 6], F32, st2)
            P.dma("sp", lambda e: e.dma_start(out=qkgc[:], in_=qkgc_d[:, :]), bC2, writes=[bC2])
            pe_f = [[sb("pe_f%d_%d" % (i, r), [128, 512], BF16, st2) for r in range(2)] for i in range(2)]; bpe_f = [[Buf("pe_f%d_%d" % (i, r)) for r in range(2)] for i in range(2)]
            pb_ = [[sb("pb_%d_%d" % (i, r), [128, 512], BF16, st2) for r in range(2)] for i in range(2)]; bpb = [[Buf("pb%d_%d" % (i, r)) for r in range(2)] for i in range(2)]
            ps_qk2 = [psum("ps_qk%d" % i, [128, 512], F32, st2) for i in range(2)]; bps_qk2 = [PB("ps_qk%d" % i) for i in range(2)]
            ps_vt2 = [psum("ps_vt%d" % i, [128, 512], F32, st2) for i in range(2)]; bps_vt2 = [PB("ps_vt%d" % i) for i in range(2)]
            ps_s = [psum("ps_s%d" % i, [128, 512], F32, st2) for i in range(2)]; bps_s = [PB("ps_s%d" % i) for i in range(2)]
            ps_o = [psum("ps_o%d" % i, [128, 512], F32, st2) for i in range(2)]; bps_o = [PB("ps_o%d" % i) for i in range(2)]
            cnt_h = 0
            for g in range(3):
                d = DILS[g]
                L = T // d
                nblk = L // 128
                wgi = g % 2
                for j in range(3):
                    c0 = j * 768 + g * 256
                    P.dma("pool", lambda e, j=j, c0=c0, wgi=wgi: e.dma_start(
                        out=wg[wgi][:, :, j * 256:(j + 1) * 256],
                        in_=w_in[:, c0:c0 + 256].rearrange("(kc p) n -> p kc n", p=128)), bwg[wgi], writes=[bwg[wgi]])
                def tile_geom(st_):
                    r = st_ // nblk
                    n = st_ % nblk
                    tok0 = n * 128 * d + r
                    return n, slice(tok0, tok0 + 127 * d + 1, d)

                def front(st_):
                    n, tsl = tile_geom(st_)
                    fi = st_ % 2
                    pqk = ps_qk2[fi]; bpqk = bps_qk2[fi]; pvt = ps_vt2[fi]; bpvt = bps_vt2[fi]
                    pvt_b = pvt[:].bitcast(BF16)

                    def mmqkv(e):
                        for kc in range(8):
                            e.matmul(pqk[:, :], lhsT=hT[:, kc, tsl], rhs=wg[wgi][:, kc, 0:512], start=(kc == 0), stop=(kc == 7))
                        for kc in range(8):
                            rr = e.matmul(pvt[:, 0:256], lhsT=hT[:, kc, tsl], rhs=wg[wgi][:, kc, 512:768], start=(kc == 0), stop=(kc == 7))
                        return rr
                    P.op("pe", mmqkv, reads=[b_hT, bwg[wgi]], writes=[bpqk, bpvt])
                    P.op("act", lambda e: e.activation(out=sq2[fi][:], in_=pqk[:, :], func=AF.Square), reads=[bpqk], writes=[bsq2[fi]])
                    P.op("dve", lambda e: e.tensor_reduce(out=ssq[fi][:], in_=sq2[fi][:].rearrange("p (h c) -> p h c", h=8), axis=AX.X, op=ALU.add), reads=[bsq2[fi]], writes=[bssq[fi]])
                    P.op("dve", lambda e: e.tensor_scalar(out=ssq[fi][:], in0=ssq[fi][:], scalar1=1.0 / 64, scalar2=1e-6, op0=ALU.mult, op1=ALU.add), reads=[bssq[fi]], writes=[bssq[fi]])
                    P.op("act", lambda e: e.activation(out=ssq[fi][:], in_=ssq[fi][:], func=AF.Ln), reads=[bssq[fi]], writes=[bssq[fi]])
                    P.op("act", lambda e: e.activation(out=ssq[fi][:], in_=ssq[fi][:], func=AF.Exp, scale=-0.5), reads=[bssq[fi]], writes=[bssq[fi]])
                    P.op("dve", lambda e: e.tensor_tensor(out=qkn[fi][:].rearrange("p (h c) -> p h c", h=8), in0=pqk[:, :].rearrange("p (h c) -> p h c", h=8),
                                                          in1=ssq[fi][:].unsqueeze(2).to_broadcast([128, 8, 64]), op=ALU.mult), reads=[bpqk, bssq[fi]], writes=[bqkn[fi]])
                    P.op("act", lambda e: e.activation(out=vg[:, st_, :], in_=pvt[:, 0:256], func=AF.Copy), reads=[bpvt], writes=[bv[st_]])

                def front2(st_):
                    fi = st_ % 2
                    pvt = ps_vt2[fi]; bpvt = bps_vt2[fi]
                    pvt_b = pvt[:].bitcast(BF16)

                    def trqk(e):
                        for j in range(4):
                            rr = e.transpose(out=pvt_b[:, 512 + j * 128:512 + (j + 1) * 128], in_=qkn[fi][:, j * 128:(j + 1) * 128], identity=ident_b[:])
                        return rr
                    P.op("pe", trqk, reads=[bqkn[fi], bC], writes=[bpvt])
                    P.op("act", lambda e: e.activation(out=qkT[:, 0:2, st_ * 128:(st_ + 1) * 128], in_=pvt_b[:, 512:768].rearrange("p (a t) -> p a t", a=2), func=AF.Copy, scale=qkgc[:, 2 * g:2 * g + 1]), reads=[bpvt, bC2], writes=[bqk[st_]])
                    P.op("act", lambda e: e.activation(out=qkT[:, 2:4, st_ * 128:(st_ + 1) * 128], in_=pvt_b[:, 768:1024].rearrange("p (a t) -> p a t", a=2), func=AF.Copy, scale=qkgc[:, 2 * g + 1:2 * g + 2]), reads=[bpvt, bC2], writes=[bqk[st_]])

                def back(st_):
                    n, tsl = tile_geom(st_)
                    has_prev = n > 0
                    c_lo = 0 if has_prev else 128
                    ci = st_ % 2
                    for r_ in range(2):
                        pb0 = r_ * 64

                        def mms(e):
                            for hl in range(2):
                                pair = hl
                                q_ap = qkT[pb0:pb0 + 64, pair, st_ * 128:(st_ + 1) * 128]
                                if has_prev:
                                    e.matmul(ps_s[r_][:, hl * 256:hl * 256 + 128], lhsT=qkT[pb0:pb0 + 64, 2 + pair, (st_ - 1) * 128:st_ * 128], rhs=q_ap, start=True, stop=True)
                                rr = e.matmul(ps_s[r_][:, hl * 256 + 128:hl * 256 + 256], lhsT=qkT[pb0:pb0 + 64, 2 + pair, st_ * 128:(st_ + 1) * 128], rhs=q_ap, start=True, stop=True)
                            return rr
                        rd = [bqk[st_]] + ([bqk[st_ - 1]] if has_prev else [])
                        P.op("pe", mms, reads=rd, writes=[bps_s[r_]], rows=(pb0, 64))
                    for r_ in range(2):
                        pf3 = pe_f[ci][r_][:].rearrange("p (h c) -> p h c", h=2)
                        pb3 = pb_[ci][r_][:].rearrange("p (h c) -> p h c", h=2)
                        ps3 = ps_s[r_][:, :].rearrange("p (h c) -> p h c", h=2)
                        E3 = Etab[:].rearrange("p (h c) -> p h c", h=12)[:, g * 4 + r_:g * 4 + r_ + 3:2, :]
                        P.op("act", lambda e: e.activation(out=pf3[:, :, c_lo:256], in_=ps3[:, :, c_lo:256], func=AF.Exp, scale=0.125), reads=[bps_s[r_]], writes=[bpe_f[ci][r_]])
                        P.op("dve" if r_ == 0 else "pool", lambda e: e.tensor_tensor(out=pb3[:, :, c_lo:256], in0=pf3[:, :, c_lo:256], in1=E3[:, :, c_lo:256], op=ALU.mult), reads=[bpe_f[ci][r_], bC2], writes=[bpb[ci][r_]])
                    for k_ in range(2):
                        def mmo(e):
                            for hl in range(2):
                                hh = 2 * k_ + hl
                                r_ = hh % 2
                                hsl = hh // 2
                                pcur = pb_[ci][r_][:, hsl * 256 + 128:hsl * 256 + 256]
                                pprev = pb_[ci][r_][:, hsl * 256:hsl * 256 + 128]
                                o_n = ps_o[k_][0:64, hl * 256:hl * 256 + 128]
                                o_d = ps_o[k_][0:64, hl * 256 + 128:hl * 256 + 256]
                                if has_prev:
                                    e.matmul(o_n, lhsT=vg[:, st_ - 1, hh * 64:(hh + 1) * 64], rhs=pprev, start=True, stop=False)
                                e.matmul(o_n, lhsT=vg[:, st_, hh * 64:(hh + 1) * 64], rhs=pcur, start=(not has_prev), stop=True)
                                if has_prev:
                                    e.matmul(o_d, lhsT=ones_b[:, :], rhs=pprev, start=True, stop=False)
                                rr = e.matmul(o_d, lhsT=ones_b[:, :], rhs=pcur, start=(not has_prev), stop=True)
                            return rr
                        rd = [bpb[ci][0], bpb[ci][1], bv[st_], bC] + ([bv[st_ - 1]] if has_prev else [])
                        P.op("pe", mmo, reads=rd, writes=[bps_o[k_]])
                        po3 = ps_o[k_][0:64, :].rearrange("p (h c) -> p h c", h=2)
                        an = accn[:, 2 * k_:2 * k_ + 2, tsl]
                        ad = accd[:, 2 * k_:2 * k_ + 2, tsl]
                        if g == 0:
                            P.op("act", lambda e: e.activation(out=an, in_=po3[:, :, 0:128], func=AF.Copy), reads=[bps_o[k_]], writes=[bacc[k_]])
                            P.op("dve", lambda e: e.tensor_copy(out=ad, in_=po3[:, :, 128:256]), reads=[bps_o[k_]], writes=[bacc[k_]])
                        else:
                            P.op("dve", lambda e: e.tensor_tensor(out=an, in0=po3[:, :, 0:128], in1=an, op=ALU.add), reads=[bps_o[k_], bacc[k_]], writes=[bacc[k_]])
                            P.op("dve", lambda e: e.tensor_tensor(out=ad, in0=po3[:, :, 128:256], in1=ad, op=ALU.add), reads=[bps_o[k_], bacc[k_]], writes=[bacc[k_]])

                front(0)
                front2(0)
                for st_ in range(16):
                    if st_ + 1 < 16:
                        front(st_ + 1)
                    back(st_)
                    if st_ + 1 < 16:
                        front2(st_ + 1)
            for hh in range(4):
                P.op("act", lambda e, hh=hh: e.activation(out=accd[:, hh, :], in_=accd[:, hh, :], func=AF.Ln), reads=[bacc[hh // 2]], writes=[bacc[hh // 2]])
                P.op("act", lambda e, hh=hh: e.activation(out=accd[:, hh, :], in_=accd[:, hh, :], func=AF.Exp, scale=-1.0), reads=[bacc[hh // 2]], writes=[bacc[hh // 2]])
                P.op("dve" if hh % 2 == 0 else "pool", lambda e, hh=hh: e.tensor_tensor(out=attT[:, hh, :], in0=accn[:, hh, :], in1=accd[:, hh, :], op=ALU.mult), reads=[bacc[hh // 2]], writes=[b_attT])
        P.barrier()
        if dbg and b == 0:
            with ExitStack() as std:
                tmp = sb("dbgtmp", [64, 4 * T], F32, std)
                bt = Buf("dbgtmp")
                P.op("dve", lambda e: e.tensor_copy(out=tmp[:], in_=attT[:].rearrange("p a t -> p (a t)")), reads=[b_attT], writes=[bt])
                P.dma("sp", lambda e: e.dma_start(out=dbg_d["att"][:, :], in_=tmp[:]), bt, reads=[bt], is_out=True)
                P.barrier()
        if stage < 3:
            stA.close()
            continue

        o_fin = sb("o_fin", [128, 4, T], BF16, stA)
        b_ofin = Buf("o_fin")
        TB = 256
        NTB = T // TB
        LAM = math.exp(-0.5)
        with ExitStack() as st3:
            w_rw = sb("w_rw", [128, 8, 1824], BF16, st3); b_wrwc = [Buf("w_rw%d" % i) for i in range(4)]
            lora = sb("lora", [128, 512], BF16, st3)
            guA = sb("guA", [128, 512], BF16, st3); guB = sb("guB", [32, 512], BF16, st3)
            b_w3 = Buf("w3small")
            rwcol = sb("rwcol", [128, 43], F32, st3); omm = sb("omm", [128, 15], F32, st3); omka = sb("omka", [128, 4], F32, st3)
            mask3 = sb("mask3", [128, 3, 128], F32, st3); mask_su = sb("mask_su", [128, 128], F32, st3); mask_sl = sb("mask_sl", [128, 128], F32, st3)
            bones = sb("bones", [128, 128], F32, st3); resetm = sb("resetm", [128, 4 * TB], F32, st3)
            b_c3 = Buf("c3")
            for (ci_, c0_, c1_) in ((3, 1536, 1824), (0, 0, 512), (1, 512, 1024), (2, 1024, 1536)):
                P.dma("pool", lambda e: e.dma_start(out=w_rw[:, :, c0_:c1_], in_=w_in[:, OFF_RW + c0_:OFF_RW + c1_].rearrange("(kc p) n -> p kc n", p=128)), b_wrwc[ci_], writes=[b_wrwc[ci_]])
            P.dma("pool", lambda e: e.dma_start(out=lora[:], in_=lora_d[:, :]), b_w3, writes=[b_w3])
            P.dma("pool", lambda e: e.dma_start(out=guA[:], in_=gup_d[0:128, :]), b_w3, writes=[b_w3])
            P.dma("pool", lambda e: e.dma_start(out=guB[:], in_=gup_d[128:160, :]), b_w3, writes=[b_w3])
            P.dma("sp", lambda e: e.dma_start(out=rwcol[:], in_=rwcol_d[:, :]), b_c3, writes=[b_c3])
            P.dma("sp", lambda e: e.dma_start(out=mask_su[:], in_=rwmask_d[:, 0:128]), b_c3, writes=[b_c3])
            P.dma("sp", lambda e: e.dma_start(out=mask3[:].rearrange("p a t -> p (a t)"), in_=rwmask_d[:, 128:512]), b_c3, writes=[b_c3])
            P.dma("sp", lambda e: e.dma_start(out=mask_sl[:], in_=rwmask_d[:, 512:640]), b_c3, writes=[b_c3])
            P.dma("sp", lambda e: e.dma_start(out=bones[:], in_=rwmask_d[:, 640:768]), b_c3, writes=[b_c3])
            P.dma("sp", lambda e: e.dma_start(out=resetm[:], in_=rwmask_d[:, 768:768 + 4 * TB]), b_c3, writes=[b_c3])
            P.op("dve", lambda e: e.tensor_scalar(out=omm[:], in0=rwcol[:, 0:15], scalar1=-1.0, scalar2=1.0, op0=ALU.mult, op1=ALU.add), reads=[b_c3], writes=[b_c3])
            P.op("dve", lambda e: e.tensor_scalar(out=omka[:], in0=rwcol[:, 27:31], scalar1=-1.0, scalar2=1.0, op0=ALU.mult, op1=ALU.add), reads=[b_c3], writes=[b_c3])
            hcol = sb("hcol", [128, 8], F32, st3)
            P.op("dve", lambda e: e.tensor_scalar(out=hcol[:], in0=rwcol[:, 15:23], scalar1=0.5, scalar2=None, op0=ALU.mult), reads=[b_c3], writes=[b_c3])

            def t2(n, w=TB, dt=F32, p=128):
                return sb(n, [p, w], dt, st3), Buf(n)
            raw = [t2("raw%d" % i, TB + 1) for i in range(2)]
            tmpm = [t2("tmpm%d" % i) for i in range(2)]
            car, b_car = t2("car", 15)
            waT, b_waT = t2("waT"); gaT, b_gaT = t2("gaT"); gbT, b_gbT = t2("gbT")
            twa, b_twa = t2("twa", TB, BF16); sgA, b_sgA = t2("sgA", TB, BF16); sgB, b_sgB = t2("sgB", TB, BF16, 32)
            def w4(n, dt=F32):
                return sb(n, [128, 4, TB], dt, st3), Buf(n)
            rT4, b_rT4 = w4("rT4"); kT4, b_kT4 = w4("kT4"); vT4, b_vT4 = w4("vT4")
            sgz4, b_sgz4 = w4("sgz4"); aT4, b_aT4 = w4("aT4"); kk4, b_kk4 = w4("kk4"); sqk4, b_sqk4 = w4("sqk4")
            kkn4, b_kkn4 = w4("kkn4"); k24, b_k24 = w4("k24"); bT4, b_bT4 = w4("bT4"); ta4, b_ta4 = w4("ta4")
            e14, b_e14 = w4("e14"); bonus4, b_bonus4 = w4("bonus4"); gT4, b_gT4 = w4("gT4")
            xc4 = kk4; b_xc4 = b_kk4; sqo4 = sqk4; b_sqo4 = b_sqk4
            raw4 = [sb("raw4_%d" % i, [128, 4, TB + 1], F32, st3) for i in range(2)]; b_raw4 = [Buf("raw4_%d" % i) for i in range(2)]
            RK4 = sb("RK4", [128, 4, 2, 2, 128], BF16, st3); b_RK4 = Buf("RK4")
            BT4, b_BT4 = w4("BT4", BF16); KT4, b_KT4 = w4("KT4", BF16); vTb4, b_vTb4 = w4("vTb4", BF16)
            TMs = [[sb("TM%d" % u, [128, 4, 128], BF16, st3) for u in range(2)]]; b_TMs = [[Buf("TM%d" % u) for u in range(2)]]
            XXs = [[sb("XX%d" % i, [128, 2, 128], BF16, st3) for i in range(4)]]; b_XXs = [[Buf("XX%d" % i) for i in range(4)]]
            UUs = [[sb("UU%d" % i, [128, 2, 128], BF16, st3) for i in range(4)]]; b_UUs = [[Buf("UU%d" % i) for i in range(4)]]
            M3s = [[sb("M3%d" % i, [128, 3, 128], BF16, st3) for i in range(4)]]; b_M3s = [[Buf("M3%d" % i) for i in range(4)]]
            RKs = [None]; b_RKs = [b_RK4]; e1s = [None]; b_e1s = [b_e14]; bonusTs = [None]; b_bonuss = [b_bonus4]; gTs = [None]; b_gTs = [b_gT4]
            chains = [(u, hd) for u in range(2) for hd in range(2)]
            Z2 = [sb("Z2%d" % i, [128, 64], BF16, st3) for i in range(4)]; b_Z2 = [Buf("Z2%d" % i) for i in range(4)]
            nW = [sb("nW%d" % i, [128, 128], BF16, st3) for i in range(4)]; b_nW = [Buf("nW%d" % i) for i in range(4)]
            RpT, b_RpT = t2("RpT")
            PhiT = sb("PhiT", [128, 4, 64], F32, st3); b_Phi = [Buf("Phi%d" % i) for i in range(8)]
            ST = [[sb("ST%d_%d" % (c, i), [128, 64], F32, st3) for i in range(2)] for c in range(4)]
            b_ST = [[[Buf("ST%d_%d_%d" % (c, i, hd)) for hd in range(2)] for i in range(2)] for c in range(4)]
            o_sb4 = sb("o_sb4", [128, 4, TB], F32, st3); b_osb4 = Buf("o_sb4")

            pr = [psum("pr%d" % i, [128, 512], F32, st3) for i in range(2)]; b_pr = [PB("pr%d" % i) for i in range(2)]
            pt = psum("pt", [128, 512], F32, st3); b_pt = PB("pt")
            pt_b = pt[:].bitcast(BF16)
            px = psum("px", [128, 512], F32, st3); b_px = PB("px")
            px_b = px[:].bitcast(BF16)
            pm = psum("pm", [128, 512], F32, st3); b_pmh = [PB("pm_h%d" % hd) for hd in range(2)]
            pc = [psum("pc%d" % i, [128, 512], F32, st3) for i in range(2)]; b_pc = [PB("pc0"), PB("pc1"), b_px, b_pt]
            po = psum("po", [128, 512], F32, st3); b_po = [PB("po_h%d" % hd) for hd in range(2)]
            cbanks = [pc[0], pc[1], px, pt]
            pcs = lambda ch: cbanks[ch][:, 0:256]
            prc = [0]

            def next_pr():
                i = prc[0] % 2
                prc[0] += 1
                return pr[i], b_pr[i]

            P.op("pool", lambda e: e.memset(car[:], 0.0), writes=[b_car])
            for c in range(4):
                P.op("pool", lambda e, c=c: e.memset(ST[c][0][:], 0.0), writes=[b_ST[c][0][0], b_ST[c][0][1]])
            rawc = [0]

            def proj_shift(tb, colbase, ncols, mu_i, out_t, out_b):
                ps_, bps_ = next_pr()
                i = rawc[0] % 2
                rawc[0] += 1
                rw_, brw_ = raw[i]
                tm_, btm_ = tmpm[i]
                n = ncols

                def mm(e):
                    for kc in range(8):
                        rr = e.matmul(ps_[0:n, 0:TB], lhsT=w_rw[:, kc, colbase:colbase + n], rhs=hT[:, kc, tb * TB:(tb + 1) * TB], start=(kc == 0), stop=(kc == 7))
                    return rr
                P.op("pe", mm, reads=[b_wrwc[min(colbase // 512, 3)], b_hT], writes=[bps_])
                P.op("act", lambda e: e.activation(out=rw_[0:n, 1:TB + 1], in_=ps_[0:n, 0:TB], func=AF.Copy), reads=[bps_], writes=[brw_])
                P.op("dve", lambda e: e.tensor_copy(out=rw_[0:n, 0:1], in_=car[0:n, mu_i:mu_i + 1]), reads=[b_car], writes=[brw_])
                P.op("pool", lambda e: e.tensor_copy(out=car[0:n, mu_i:mu_i + 1], in_=rw_[0:n, TB:TB + 1]), reads=[brw_], writes=[b_car])
                P.op("act", lambda e: e.activation(out=tm_[0:n, :], in_=rw_[0:n, 0:TB], func=AF.Copy, scale=rwcol[0:n, mu_i:mu_i + 1]), reads=[brw_, b_c3], writes=[btm_])
                P.op("dve", lambda e: e.scalar_tensor_tensor(out=out_t[0:n, :], in0=rw_[0:n, 1:TB + 1], scalar=omm[0:n, mu_i:mu_i + 1], in1=tm_[0:n, :], op0=ALU.mult, op1=ALU.add), reads=[brw_, btm_, b_c3], writes=[out_b])

            def bc4(col0):
                return lambda t_: t_[:, col0:col0 + 4].unsqueeze(2).to_broadcast([128, 4, TB])
            fl = lambda t_: t_[:].rearrange("p c t -> p (c t)")
            rawk = [0]

            def stageAW(tb):
                proj_shift(tb, 1536, 128, 12, waT, b_waT)
                proj_shift(tb, 1664, 128, 13, gaT, b_gaT)
                proj_shift(tb, 1792, 32, 14, gbT, b_gbT)
                P.op("act", lambda e: e.activation(out=twa[0:64, :], in_=waT[0:64, :], func=AF.Tanh), reads=[b_waT], writes=[b_twa])
                P.op("act", lambda e: e.activation(out=twa[64:128, :], in_=waT[64:128, :], func=AF.Copy), reads=[b_waT], writes=[b_twa])
                P.op("act", lambda e: e.activation(out=gaT[:], in_=gaT[:], func=AF.Tanh, scale=0.5), reads=[b_gaT], writes=[b_gaT])
                P.op("act", lambda e: e.activation(out=gbT[0:32, :], in_=gbT[0:32, :], func=AF.Tanh, scale=0.5), reads=[b_gbT], writes=[b_gbT])
                P.op("dve", lambda e: e.tensor_scalar(out=sgA[:], in0=gaT[:], scalar1=0.5, scalar2=0.5, op0=ALU.mult, op1=ALU.add), reads=[b_gaT], writes=[b_sgA])
                P.op("dve", lambda e: e.tensor_scalar(out=sgB[:], in0=gbT[0:32, :], scalar1=0.5, scalar2=0.5, op0=ALU.mult, op1=ALU.add), reads=[b_gbT], writes=[b_sgB])
                for (base, ci, out4, bout) in ((0, 0, rT4, b_rT4), (512, 4, kT4, b_kT4), (1024, 8, vT4, b_vT4)):
                    rw4 = raw4[rawk[0] % 2]; brw4 = b_raw4[rawk[0] % 2]
                    rawk[0] += 1
                    for c in range(4):
                        ps_, bps_ = next_pr()

                        def mm(e):
                            for kc in range(8):
                                rr = e.matmul(ps_[:, 0:TB], lhsT=w_rw[:, kc, base + c * 128:base + (c + 1) * 128], rhs=hT[:, kc, tb * TB:(tb + 1) * TB], start=(kc == 0), stop=(kc == 7))
                            return rr
                        P.op("pe", mm, reads=[b_wrwc[base // 512], b_hT], writes=[bps_])
                        P.op("act", lambda e: e.activation(out=rw4[:, c, 1:TB + 1], in_=ps_[:, 0:TB], func=AF.Copy), reads=[bps_], writes=[brw4])
                    P.op("dve", lambda e: e.tensor_copy(out=rw4[:, :, 0:1], in_=car[:, ci:ci + 4].unsqueeze(2)), reads=[b_car], writes=[brw4])
                    P.op("pool", lambda e: e.tensor_copy(out=car[:, ci:ci + 4].unsqueeze(2), in_=rw4[:, :, TB:TB + 1]), reads=[brw4], writes=[b_car])
                    P.op("pool", lambda e: e.tensor_tensor(out=ta4[:], in0=rw4[:, :, 0:TB], in1=bc4(ci)(rwcol), op=ALU.mult), reads=[brw4, b_c3], writes=[b_ta4])
                    P.op("dve", lambda e: e.tensor_tensor(out=out4[:], in0=rw4[:, :, 1:TB + 1], in1=bc4(ci)(omm), op=ALU.mult), reads=[brw4, b_c3], writes=[bout])
                    P.op("dve", lambda e: e.tensor_tensor(out=out4[:], in0=out4[:], in1=ta4[:], op=ALU.add), reads=[b_ta4], writes=[bout])
                for c in range(4):
                    pz, bpz = next_pr()
                    P.op("pe", lambda e: e.matmul(pz[:, 0:TB], lhsT=lora[0:64, c * 128:(c + 1) * 128], rhs=twa[0:64, :], start=True, stop=True), reads=[b_w3, b_twa], writes=[bpz], rows=(0, 64))
                    P.op("act", lambda e: e.activation(out=sgz4[:, c, :], in_=pz[:, 0:TB], func=AF.Tanh, bias=hcol[:, c:c + 1], scale=0.5), reads=[bpz, b_c3], writes=[b_sgz4])
                P.op("dve", lambda e: e.tensor_scalar(out=fl(sgz4), in0=fl(sgz4), scalar1=0.5, scalar2=0.5, op0=ALU.mult, op1=ALU.add), reads=[b_sgz4], writes=[b_sgz4])
                for c in range(4):
                    pa, bpa = next_pr()
                    P.op("pe", lambda e: e.matmul(pa[:, 0:TB], lhsT=lora[64:128, c * 128:(c + 1) * 128], rhs=twa[64:128, :], start=True, stop=True), reads=[b_w3, b_twa], writes=[bpa], rows=(64, 64))
                    P.op("act", lambda e: e.activation(out=aT4[:, c, :], in_=pa[:, 0:TB], func=AF.Tanh, bias=hcol[:, 4 + c:5 + c], scale=0.5), reads=[bpa, b_c3], writes=[b_aT4])
                P.op("pool", lambda e: e.tensor_scalar(out=fl(aT4), in0=fl(aT4), scalar1=0.5, scalar2=0.5, op0=ALU.mult, op1=ALU.add), reads=[b_aT4], writes=[b_aT4])
                for c in range(4):
                    pg, bpg = next_pr()

                    def mmg(e):
                        e.matmul(pg[:, 0:TB], lhsT=guA[:, c * 128:(c + 1) * 128], rhs=sgA[:], start=True, stop=False)
                        return e.matmul(pg[:, 0:TB], lhsT=guB[0:32, c * 128:(c + 1) * 128], rhs=sgB[0:32, :], start=False, stop=True)
                    P.op("pe", mmg, reads=[b_w3, b_sgA, b_sgB], writes=[bpg])
                    P.op("act", lambda e: e.activation(out=gT4[:, c, :], in_=pg[:, 0:TB], func=AF.Copy), reads=[bpg], writes=[b_gT4])
                for c in range(4):
                    P.op("act", lambda e: e.activation(out=kk4[:, c, :], in_=kT4[:, c, :], func=AF.Copy, scale=rwcol[:, 23 + c:24 + c]), reads=[b_kT4, b_c3], writes=[b_kk4])
                    P.op("act", lambda e: e.activation(out=sqk4[:, c, :], in_=kT4[:, c, :], func=AF.Square, scale=rwcol[:, 23 + c:24 + c]), reads=[b_kT4, b_c3], writes=[b_sqk4])
                for hf in range(2):
                    pn, bpn = next_pr()
                    P.op("pe", lambda e: e.matmul(pn[:, :], lhsT=bones[:], rhs=fl(sqk4)[:, hf * 512:(hf + 1) * 512], start=True, stop=True), reads=[b_c3, b_sqk4], writes=[bpn])
                    P.op("dve", lambda e: e.tensor_scalar(out=fl(sqk4)[:, hf * 512:(hf + 1) * 512], in0=pn[:, :], scalar1=1e-18, scalar2=None, op0=ALU.max), reads=[bpn], writes=[b_sqk4])
                P.op("act", lambda e: e.activation(out=fl(sqk4), in_=fl(sqk4), func=AF.Ln), reads=[b_sqk4], writes=[b_sqk4])
                P.op("act", lambda e: e.activation(out=fl(sqk4), in_=fl(sqk4), func=AF.Exp, scale=-0.5), reads=[b_sqk4], writes=[b_sqk4])
                P.op("dve", lambda e: e.tensor_tensor(out=kkn4[:], in0=kk4[:], in1=sqk4[:], op=ALU.mult), reads=[b_kk4, b_sqk4], writes=[b_kkn4])
                for c in range(4):
                    P.op("act", lambda e: e.activation(out=kk4[:, c, :], in_=aT4[:, c, :], func=AF.Identity, scale=rwcol[:, 27 + c:28 + c], bias=omka[:, c:c + 1]), reads=[b_aT4, b_c3, b_kkn4], writes=[b_kk4])
                P.op("pool", lambda e: e.tensor_tensor(out=k24[:], in0=kT4[:], in1=kk4[:], op=ALU.mult), reads=[b_kT4, b_kk4], writes=[b_k24])
                P.op("dve", lambda e: e.tensor_tensor(out=bT4[:], in0=kkn4[:], in1=aT4[:], op=ALU.mult), reads=[b_kkn4, b_aT4], writes=[b_bT4])
                P.op("dve", lambda e: e.tensor_tensor(out=sqk4[:], in0=rT4[:], in1=bc4(31)(rwcol), op=ALU.mult), reads=[b_rT4, b_c3, b_kkn4], writes=[b_sqk4])
                P.op("dve", lambda e: e.tensor_tensor(out=sqk4[:], in0=sqk4[:], in1=k24[:], op=ALU.mult), reads=[b_k24], writes=[b_sqk4])
                for hf in range(2):
                    pbn, bpbn = next_pr()
                    P.op("pe", lambda e: e.matmul(pbn[:, :], lhsT=bones[:], rhs=fl(sqk4)[:, hf * 512:(hf + 1) * 512], start=True, stop=True), reads=[b_c3, b_sqk4], writes=[bpbn])
                    P.op("dve", lambda e: e.tensor_tensor(out=fl(bonus4)[:, hf * 512:(hf + 1) * 512], in0=pbn[:, :], in1=fl(vT4)[:, hf * 512:(hf + 1) * 512], op=ALU.mult), reads=[bpbn, b_vT4], writes=[b_bonus4])
                P.op("dve", lambda e: e.tensor_tensor_scan(out=fl(ta4), data0=resetm[:], data1=fl(sgz4), initial=0.0, op0=ALU.mult, op1=ALU.add), reads=[b_c3, b_sgz4], writes=[b_ta4])
                P.op("act", lambda e: e.activation(out=fl(e14), in_=fl(ta4), func=AF.Exp, scale=-LAM), reads=[b_ta4], writes=[b_e14])
                P.op("pool", lambda e: e.tensor_tensor(out=sgz4[:], in0=ta4[:], in1=sgz4[:], op=ALU.subtract), reads=[b_ta4], writes=[b_sgz4])
                P.op("act", lambda e: e.activation(out=fl(sgz4), in_=fl(sgz4), func=AF.Exp, scale=-LAM), reads=[b_sgz4], writes=[b_sgz4])
                P.op("act", lambda e: e.activation(out=fl(ta4), in_=fl(ta4), func=AF.Exp, scale=LAM), reads=[b_sgz4, b_e14], writes=[b_ta4])
                v4 = lambda t_: t_[:].rearrange("p c (u t) -> p c u t", u=2)
                P.op("pool", lambda e: e.tensor_tensor(out=RK4[:, :, :, 0, :], in0=v4(kkn4), in1=v4(sgz4), op=ALU.mult), reads=[b_kkn4, b_sgz4], writes=[b_RK4])
                P.op("dve", lambda e: e.tensor_tensor(out=RK4[:, :, :, 1, :], in0=v4(rT4), in1=v4(e14), op=ALU.mult), reads=[b_rT4, b_e14], writes=[b_RK4])
                P.op("pool", lambda e: e.tensor_tensor(out=BT4[:], in0=bT4[:], in1=ta4[:], op=ALU.mult), reads=[b_bT4, b_ta4], writes=[b_BT4])
                P.op("pool", lambda e: e.tensor_tensor(out=KT4[:], in0=k24[:], in1=ta4[:], op=ALU.mult), reads=[b_k24, b_ta4], writes=[b_KT4])
                P.op("act", lambda e: e.activation(out=fl(vTb4), in_=fl(vT4), func=AF.Copy), reads=[b_vT4], writes=[b_vTb4])

            def stageA(tb, c, sl):
                RK = RK4[:, c]; b_RK = b_RK4
                BT = BT4[:, c, :]; KT = KT4[:, c, :]; vTb = vTb4[:, c, :]
                b_BT = b_BT4; b_KT = b_KT4; b_vTb = b_vTb4
                RKs[0] = RK; e1s[0] = e14[:, c, :]; bonusTs[0] = bonus4[:, c, :]; gTs[0] = gT4[:, c, :]
                TM = TMs[sl]; b_TM = b_TMs[sl]; XX = XXs[sl]; b_XX = b_XXs[sl]
                UU = UUs[sl]; b_UU = b_UUs[sl]; M3 = M3s[sl]; b_M3 = b_M3s[sl]
                for u in range(2):
                    usl = slice(u * 128, (u + 1) * 128)

                    tb_, btb_ = ((pt_b, b_pt), (px_b, b_px))[u]

                    def trs(e):
                        e.transpose(out=tb_[:, 0:128], in_=RK[:, u, 0, :], identity=ident_b[:])
                        e.transpose(out=tb_[:, 128:256], in_=BT[:, usl], identity=ident_b[:])
                        e.transpose(out=tb_[:, 256:384], in_=KT[:, usl], identity=ident_b[:])
                        return e.transpose(out=tb_[:, 384:512], in_=vTb[:, usl], identity=ident_b[:])
                    P.op("pe", trs, reads=[b_RK, b_BT, b_KT, b_vTb, bC], writes=[btb_])
                    P.op("act" if u == 0 else "dve", (lambda e: e.activation(out=TM[u][:].rearrange("p a t -> p (a t)"), in_=tb_[:, 0:512], func=AF.Copy)) if u == 0 else (lambda e: e.tensor_copy(out=TM[u][:].rearrange("p a t -> p (a t)"), in_=tb_[:, 0:512])), reads=[btb_], writes=[b_TM[u]])
                    yield
                xbanks = [(px, b_px), (pc[0], b_pc[0]), (pc[1], b_pc[1]), (pm, b_pmh[0])]
                x3slots = [(pt[:, 256:384], b_pt), (pt[:, 384:512], b_pt), (po[:, 0:128], b_po[0]), (po[:, 128:256], b_po[0])]
                for ch in (0, 2, 1, 3):
                    u, hd = chains[ch]
                    pb0 = hd * 64
                    usl = slice(u * 128, (u + 1) * 128)
                    xb, bxb = xbanks[ch]
                    x3, bx3 = x3slots[ch]
                    wr = [bxb, bx3] + ([b_pmh[1]] if ch == 3 else []) + ([b_po[1]] if ch >= 2 else [])

                    def cross(e):
                        rk = RK[pb0:pb0 + 64, u, :, :].rearrange("p a t -> p (a t)")
                        e.matmul(xb[:, 0:256], lhsT=BT[pb0:pb0 + 64, usl], rhs=rk, start=True, stop=True)
                        e.matmul(xb[:, 256:512], lhsT=KT[pb0:pb0 + 64, usl], rhs=rk, start=True, stop=True)
                        return e.matmul(x3, lhsT=RK[pb0:pb0 + 64, u, 0, :], rhs=BT[pb0:pb0 + 64, usl], start=True, stop=True)
                    P.op("pe", cross, reads=[b_RK, b_BT, b_KT], writes=wr, rows=(pb0, 64))
                    P.op("dve", lambda e: e.tensor_tensor(out=XX[ch][:, 0, :], in0=xb[:, 0:128], in1=mask_su[:], op=ALU.mult), reads=[bxb, b_c3] + ([b_pmh[1]] if ch == 3 else []), writes=[b_XX[ch]])
                    P.op("dve", lambda e: e.tensor_tensor(out=M3[ch][:].rearrange("p a t -> p (a t)"), in0=xb[:, 128:512], in1=mask3[:].rearrange("p a t -> p (a t)"), op=ALU.mult), reads=[bxb, b_c3] + ([b_pmh[1]] if ch == 3 else []), writes=[b_M3[ch]])
                    P.op("dve", lambda e: e.tensor_tensor(out=XX[ch][:, 1, :], in0=x3, in1=mask_sl[:], op=ALU.mult), reads=[bx3] + ([b_po[1]] if ch >= 2 else []) + [b_c3], writes=[b_XX[ch]])
                    P.op("pool", lambda e: e.tensor_tensor(out=UU[ch][:], in0=ident_f[:].unsqueeze(1).to_broadcast([128, 2, 128]), in1=XX[ch][:], op=ALU.subtract), reads=[bC, b_XX[ch]], writes=[b_UU[ch]])
                    yield

            def stageB(tb, c, sl):
                RK = RKs[sl]; b_RK = b_RKs[sl]; TM = TMs[sl]; b_TM = b_TMs[sl]; XX = XXs[sl]; b_XX = b_XXs[sl]
                UU = UUs[sl]; b_UU = b_UUs[sl]; M3 = M3s[sl]; b_M3 = b_M3s[sl]
                e1 = e1s[sl]; b_e1 = b_e1s[sl]; bonusT = bonusTs[sl]; b_bonus = b_bonuss[sl]; gT = gTs[sl]; b_gT = b_gTs[sl]
                for k in range(1, 6):
                    last = (k == 5)
                    for ch in range(4):
                        def sqr(e):
                            rr = e.matmul(pcs(ch)[:, 0:128], lhsT=XX[ch][:, 1, :], rhs=XX[ch][:, 0, :], start=True, stop=True)
                            if not last:
                                rr = e.matmul(pcs(ch)[:, 128:256], lhsT=XX[ch][:, 0, :], rhs=XX[ch][:, 1, :], start=True, stop=True)
                            return rr
                        P.op("pe", sqr, reads=[b_XX[ch]], writes=[b_pc[ch]])
                        if last:
                            P.op("act", lambda e: e.activation(out=XX[ch][:, 0, :], in_=pcs(ch)[:, 0:128], func=AF.Copy), reads=[b_pc[ch]], writes=[b_XX[ch]])
                        else:
                            P.op("act", lambda e: e.activation(out=XX[ch][:].rearrange("p a t -> p (a t)"), in_=pcs(ch)[:, 0:256], func=AF.Copy), reads=[b_pc[ch]], writes=[b_XX[ch]])
                        yield
                    for ch in range(4):
                        def app(e):
                            rr = e.matmul(pcs(ch)[:, 0:128], lhsT=UU[ch][:, 1, :], rhs=XX[ch][:, 0, :], start=True, stop=True)
                            if not last:
                                rr = e.matmul(pcs(ch)[:, 128:256], lhsT=XX[ch][:, 0, :], rhs=UU[ch][:, 1, :], start=True, stop=True)
                            return rr
                        P.op("pe", app, reads=[b_XX[ch], b_UU[ch]], writes=[b_pc[ch]])
                        if last:
                            P.op("dve", lambda e: e.tensor_tensor(out=UU[ch][:, 0, :], in0=pcs(ch)[:, 0:128], in1=UU[ch][:, 0, :], op=ALU.add), reads=[b_pc[ch], b_UU[ch]], writes=[b_UU[ch]])
                        else:
                            P.op("dve", lambda e: e.tensor_tensor(out=UU[ch][:].rearrange("p a t -> p (a t)"), in0=pcs(ch)[:, 0:256], in1=UU[ch][:].rearrange("p a t -> p (a t)"), op=ALU.add), reads=[b_pc[ch], b_UU[ch]], writes=[b_UU[ch]])
                        yield
                for ch, (u, hd) in enumerate(chains):
                    pb0 = hd * 64
                    P.op("pe", lambda e: e.matmul(pcs(ch)[:, 0:64], lhsT=M3[ch][:, 1, :], rhs=TM[u][:, 3, pb0:pb0 + 64], start=True, stop=True), reads=[b_M3[ch], b_TM[u]], writes=[b_pc[ch]])
                    P.op("act", lambda e: e.activation(out=Z2[ch][:], in_=pcs(ch)[:, 0:64], func=AF.Copy), reads=[b_pc[ch]], writes=[b_Z2[ch]])
                    yield
                for ch, (u, hd) in enumerate(chains):
                    pb0 = hd * 64

                    def mmw(e):
                        e.matmul(pcs(ch)[:, 128:192], lhsT=UU[ch][:, 0, :], rhs=TM[u][:, 0, pb0:pb0 + 64], start=True, stop=True)
                        return e.matmul(pcs(ch)[:, 192:256], lhsT=UU[ch][:, 0, :], rhs=Z2[ch][:], start=True, stop=True)
                    P.op("pe", mmw, reads=[b_UU[ch], b_TM[u], b_Z2[ch]], writes=[b_pc[ch]])
                    P.op("act", lambda e: e.activation(out=nW[ch][:], in_=pcs(ch)[:, 128:256], func=AF.Copy, scale=-1.0), reads=[b_pc[ch]], writes=[b_nW[ch]])
                    yield
                for ch, (u, hd) in enumerate(chains):
                    pb0 = hd * 64
                    P.op("pe", lambda e: e.matmul(cbanks[ch][pb0:pb0 + 64, 0:128], lhsT=nW[ch][:, 0:64], rhs=M3[ch][:, 0, :], start=True, stop=True), reads=[b_nW[ch], b_M3[ch]], writes=[b_pc[ch]])
                    P.op("dve", lambda e: e.tensor_tensor(out=RpT[pb0:pb0 + 64, u * 128:(u + 1) * 128], in0=cbanks[ch][pb0:pb0 + 64, 0:128], in1=RK[pb0:pb0 + 64, u, 1, :], op=ALU.add), reads=[b_pc[ch], b_RK], writes=[b_RpT])
                    yield
                for s in (0, 2, 1, 3):
                    u = s // 2; t0 = (s % 2) * 64
                    for hd in range(2):
                        pb0 = hd * 64
                        ch = u * 2 + hd
                        P.op("pe", lambda e: e.matmul(cbanks[ch][pb0:pb0 + 64, 256 + (s % 2) * 64:256 + (s % 2) * 64 + 64], lhsT=nW[ch][t0:t0 + 64, 0:64], rhs=TM[u][t0:t0 + 64, 1, pb0:pb0 + 64], start=True, stop=True), reads=[b_nW[ch], b_TM[u]], writes=[b_pc[ch]], rows=(t0, 64))
                        P.op("dve", lambda e: e.tensor_tensor(out=PhiT[pb0:pb0 + 64, s, :], in0=cbanks[ch][pb0:pb0 + 64, 256 + (s % 2) * 64:256 + (s % 2) * 64 + 64], in1=ident_f[pb0:pb0 + 64, pb0:pb0 + 64], op=ALU.add), reads=[b_pc[ch], bC], writes=[b_Phi[s * 2 + hd]])
                        yield
                for s in range(4):
                    u = s // 2; s2 = s % 2; t0 = s2 * 64
                    gs = tb * 4 + s
                    cur = gs % 2; nxt = (gs + 1) % 2
                    for hd in range(2):
                        pb0 = hd * 64
                        ch = u * 2 + hd
                        o_ap = po[pb0:pb0 + 64, s * 64:(s + 1) * 64]

                        def mo1(e):
                            e.matmul(o_ap, lhsT=TM[u][t0:t0 + 64, 3, pb0:pb0 + 64], rhs=M3[ch][t0:t0 + 64, 2, s2 * 64:(s2 + 1) * 64], start=True, stop=False)
                            return e.matmul(o_ap, lhsT=nW[ch][t0:t0 + 64, 64:128], rhs=M3[ch][t0:t0 + 64, 0, s2 * 64:(s2 + 1) * 64], start=False, stop=False)
                        s_ap = pm[pb0:pb0 + 64, 256 + (s % 2) * 64:256 + (s % 2) * 64 + 64]

                        def ms1(e):
                            e.matmul(s_ap, lhsT=TM[u][t0:t0 + 64, 2, pb0:pb0 + 64], rhs=TM[u][t0:t0 + 64, 3, pb0:pb0 + 64], start=True, stop=False)
                            return e.matmul(s_ap, lhsT=TM[u][t0:t0 + 64, 1, pb0:pb0 + 64], rhs=nW[ch][t0:t0 + 64, 64:128], start=False, stop=False)
                        P.op("pe", mo1, reads=[b_TM[u], b_M3[ch], b_nW[ch]], writes=[b_po[hd]], rows=(t0, 64))
                        P.op("pe", ms1, reads=[b_TM[u], b_nW[ch]], writes=[b_pmh[hd]], rows=(t0, 64))
                        P.op("pe", lambda e: e.matmul(o_ap, lhsT=ST[c][cur][pb0:pb0 + 64, :], rhs=RpT[pb0:pb0 + 64, s * 64:(s + 1) * 64], start=False, stop=True),
                             reads=[b_ST[c][cur][hd], b_RpT], writes=[b_po[hd]], rows=(pb0, 64))
                        P.op("pe", lambda e: e.matmul(s_ap, lhsT=PhiT[pb0:pb0 + 64, s, :], rhs=ST[c][cur][pb0:pb0 + 64, :], start=False, stop=True),
                             reads=[b_Phi[s * 2 + hd], b_ST[c][cur][hd]], writes=[b_pmh[hd]], rows=(pb0, 64))
                        P.op("act", lambda e: e.activation(
                            out=ST[c][nxt][pb0:pb0 + 64, :], in_=pm[pb0:pb0 + 64, 256 + (s % 2) * 64:256 + (s % 2) * 64 + 64],
                            func=AF.Copy, scale=e1[pb0:pb0 + 64, s * 64 + 63:s * 64 + 64]),
                            reads=[b_pmh[hd], b_e1], writes=[b_ST[c][nxt][hd]])
                        yield
                P.op("act", lambda e: e.activation(out=o_sb4[:, c, :], in_=po[:, 0:TB], func=AF.Copy), reads=[b_po[0], b_po[1]], writes=[b_osb4])
                yield

            def stageGN(tb):
                for hf in range(2):
                    hs = slice(hf * 512, (hf + 1) * 512)
                    p1, bp1 = next_pr()
                    P.op("pe", lambda e: e.matmul(p1[:, :], lhsT=bones[:], rhs=fl(o_sb4)[:, hs], start=True, stop=True), reads=[b_c3, b_osb4], writes=[bp1])
                    P.op("dve", lambda e: e.scalar_tensor_tensor(out=fl(xc4)[:, hs], in0=p1[:, :], scalar=-1.0 / 64, in1=fl(o_sb4)[:, hs], op0=ALU.mult, op1=ALU.add), reads=[bp1, b_osb4], writes=[b_xc4])
                P.op("pool", lambda e: e.tensor_tensor(out=sqo4[:], in0=xc4[:], in1=xc4[:], op=ALU.mult), reads=[b_xc4], writes=[b_sqo4])
                for hf in range(2):
                    hs = slice(hf * 512, (hf + 1) * 512)
                    p2, bp2 = next_pr()
                    P.op("pe", lambda e: e.matmul(p2[:, :], lhsT=bones[:], rhs=fl(sqo4)[:, hs], start=True, stop=True), reads=[b_c3, b_sqo4], writes=[bp2])
                    P.op("dve", lambda e: e.tensor_scalar(out=fl(sqo4)[:, hs], in0=p2[:, :], scalar1=1.0 / 64, scalar2=64e-5, op0=ALU.mult, op1=ALU.add), reads=[bp2], writes=[b_sqo4])
                P.op("act", lambda e: e.activation(out=fl(sqo4), in_=fl(sqo4), func=AF.Ln), reads=[b_sqo4], writes=[b_sqo4])
                P.op("act", lambda e: e.activation(out=fl(sqo4), in_=fl(sqo4), func=AF.Exp, scale=-0.5), reads=[b_sqo4], writes=[b_sqo4])
                P.op("dve", lambda e: e.tensor_tensor(out=xc4[:], in0=xc4[:], in1=sqo4[:], op=ALU.mult), reads=[b_sqo4], writes=[b_xc4])
                P.op("pool", lambda e: e.tensor_tensor(out=xc4[:], in0=xc4[:], in1=bc4(35)(rwcol), op=ALU.mult), reads=[b_c3], writes=[b_xc4])
                P.op("pool", lambda e: e.tensor_tensor(out=xc4[:], in0=xc4[:], in1=bc4(39)(rwcol), op=ALU.add), reads=[b_c3], writes=[b_xc4])
                P.op("dve", lambda e: e.tensor_tensor(out=xc4[:], in0=xc4[:], in1=bonus4[:], op=ALU.add), reads=[b_bonus4], writes=[b_xc4])
                P.op("dve", lambda e: e.tensor_tensor(out=o_fin[:, :, tb * TB:(tb + 1) * TB], in0=xc4[:], in1=gT4[:], op=ALU.mult), reads=[b_xc4, b_gT4], writes=[b_ofin])

            for tb in range(NTB):
                stageAW(tb)
                for c in range(4):
                    for _ in stageA(tb, c, 0):
                        pass
                    for _ in stageB(tb, c, 0):
                        pass
                stageGN(tb)
        P.barrier()
        if dbg and b == 0:
            with ExitStack() as std:
                tmp = sb("dbgtmp2", [128, 4 * T], F32, std)
                bt = Buf("dbgtmp2")
                P.op("dve", lambda e: e.tensor_copy(out=tmp[:], in_=o_fin[:].rearrange("p a t -> p (a t)")), reads=[b_ofin], writes=[bt])
                P.dma("sp", lambda e: e.dma_start(out=dbg_d["o_rw"][:, :], in_=tmp[:]), bt, reads=[bt], is_out=True)
                P.barrier()
        if stage < 4:
            stA.close()
            continue

        with ExitStack() as st4:
            w_gt = sb("w_gt", [128, 8, 2048], BF16, st4)
            w_o = sb("w_o", [128, 8, 1024], BF16, st4)
            w_ao = sb("w_ao", [64, 4, 1024], BF16, st4)
            w_ro = sb("w_ro", [128, 4, 1024], BF16, st4)
            b_w4 = Buf("w4"); b_wo4 = Buf("wo4"); b_wg4 = [Buf("wg4_%d" % i) for i in range(4)]
            P.dma("pool", lambda e: e.dma_start(out=w_ao[:, :, :], in_=wao_d.rearrange("(h e) n -> e h n", e=64)), b_w4, writes=[b_w4])
            P.dma("pool", lambda e: e.dma_start(out=w_ro[:, :, :], in_=wro_d.rearrange("(c p) n -> p c n", p=128)), b_w4, writes=[b_w4])
            for q4 in range(4):
                for off in (0, 1024):
                    c0 = off + q4 * 256
                    P.dma("pool", lambda e: e.dma_start(out=w_gt[:, :, c0:c0 + 256], in_=w_in[:, OFF_GATE + c0:OFF_GATE + c0 + 256].rearrange("(kc p) n -> p kc n", p=128)), b_wg4[q4], writes=[b_wg4[q4]])
            P.dma("pool", lambda e: e.dma_start(out=w_o[:, :, :], in_=wout_d.rearrange("(kc p) n -> p kc n", p=128)), b_wo4, writes=[b_wo4])
            g1s = sb("g1s", [128, 512], F32, st4); b_g1s = Buf("g1s")
            g2s = sb("g2s", [128, 512], F32, st4); b_g2s = Buf("g2s")
            m1 = sb("m1", [128, 512], F32, st4); b_m1 = Buf("m1")
            m2 = sb("m2", [128, 512], F32, st4); b_m2 = Buf("m2")
            mixedT = sb("mixedT", [128, 8, 512], BF16, st4); b_mix = [Buf("mix%d" % i) for i in range(8)]
            xr = [sb("xr%d" % i, [128, D], F32, st4) for i in range(2)]; b_xr = [Buf("xr%d" % i) for i in range(2)]
            x1t = [sb("x1t%d" % i, [128, D], F32, st4) for i in range(2)]; b_x1t = [Buf("x1t%d" % i) for i in range(2)]
            p_att = psum("p_att", [128, 512], F32, st4); b_patt = PB("p_att")
            p_rw = psum("p_rw", [128, 512], F32, st4); b_prw = PB("p_rw")
            p_g1 = psum("p_g1", [128, 512], F32, st4); b_pg1 = PB("p_g1")
            p_g2 = psum("p_g2", [128, 512], F32, st4); b_pg2 = PB("p_g2")
            p_out = [psum("p_out%d" % i, [128, 512], F32, st4) for i in range(2)]; b_pout = [PB("p_out%d" % i) for i in range(2)]
            for tq in range(4):
                qsl = slice(tq * 512, (tq + 1) * 512)
                for fc in range(8):
                    fsl = slice(fc * 128, (fc + 1) * 128)

                    def mm_g(e, pg, off):
                        for kc in range(8):
                            rr = e.matmul(pg[:, :], lhsT=w_gt[:, kc, off + fc * 128:off + (fc + 1) * 128], rhs=hT[:, kc, qsl], start=(kc == 0), stop=(kc == 7))
                        return rr
                    P.op("pe", lambda e: mm_g(e, p_g1, 0), reads=[b_wg4[fc // 2], b_hT], writes=[b_pg1])
                    P.op("pe", lambda e: mm_g(e, p_g2, 1024), reads=[b_wg4[fc // 2], b_hT], writes=[b_pg2])

                    def mm_rw(e):
                        for c in range(4):
                            rr = e.matmul(p_rw[:, :], lhsT=w_ro[:, c, fsl], rhs=o_fin[:, c, qsl], start=(c == 0), stop=(c == 3))
                        return rr
                    P.op("pe", mm_rw, reads=[b_w4, b_ofin], writes=[b_prw])

                    def mm_att(e):
                        for hh in range(4):
                            rr = e.matmul(p_att[:, :], lhsT=w_ao[0:64, hh, fsl], rhs=attT[0:64, hh, qsl], start=(hh == 0), stop=(hh == 3))
                        return rr
                    P.op("pe", mm_att, reads=[b_w4, b_attT], writes=[b_patt], rows=(0, 64))
                    P.op("act", lambda e: e.activation(out=g1s[:], in_=p_g1[:, :], func=AF.Sigmoid), reads=[b_pg1], writes=[b_g1s])
                    P.op("act", lambda e: e.activation(out=g2s[:], in_=p_g2[:, :], func=AF.Sigmoid), reads=[b_pg2], writes=[b_g2s])
                    P.op("dve", lambda e: e.tensor_tensor(out=m1[:], in0=p_att[:, :], in1=g1s[:], op=ALU.mult), reads=[b_patt, b_g1s], writes=[b_m1])
                    P.op("dve", lambda e: e.tensor_tensor(out=m2[:], in0=p_rw[:, :], in1=g2s[:], op=ALU.mult), reads=[b_prw, b_g2s], writes=[b_m2])
                    P.op("pool", lambda e: e.tensor_tensor(out=mixedT[:, fc, :], in0=m1[:], in1=m2[:], op=ALU.add), reads=[b_m1, b_m2], writes=[b_mix[fc]])
                for t4 in range(4):
                    tt = tq * 4 + t4
                    i2 = tt % 2
                    P.dma("sp", lambda e: e.dma_start(out=xr[i2][:], in_=x_d[b, tt * 128:(tt + 1) * 128, :]), b_xr[i2], writes=[b_xr[i2]])
                    for half in range(2):
                        def mm_o(e):
                            for kc in range(8):
                                rr = e.matmul(p_out[half][:, :], lhsT=mixedT[:, kc, t4 * 128:(t4 + 1) * 128], rhs=w_o[:, kc, half * 512:(half + 1) * 512], start=(kc == 0), stop=(kc == 7))
                            return rr
                        P.op("pe", mm_o, reads=b_mix + [b_wo4], writes=[b_pout[half]])
                        P.op("dve", lambda e: e.tensor_tensor(out=x1t[i2][:, half * 512:(half + 1) * 512], in0=p_out[half][:, :], in1=xr[i2][:, half * 512:(half + 1) * 512], op=ALU.add),
                             reads=[b_pout[half], b_xr[i2]], writes=[b_x1t[i2]])
                    P.dma("sp", lambda e: e.dma_start(out=y_d[b, tt * 128:(tt + 1) * 128, :], in_=x1t[i2][:]), b_x1t[i2], reads=[b_x1t[i2]], is_out=True)
        stA.close()
        P.barrier()
        if stage < 5:
            continue

        with ExitStack() as st5:
            x1s = sb("x1s", [128, 16, D], F32, st5); b_x1s = [Buf("x1s%d" % i) for i in range(16)]
            h2T = sb("h2T", [128, 8, T], BF16, st5); b_h2T = Buf("h2T")
            w_rt = sb("w_rt", [128, 8, 36], F32, st5); rbias = sb("rbias", [128, 36], F32, st5); b_c5 = Buf("c5")
            P.dma("sp", lambda e: e.dma_start(out=w_rt[:, :, :], in_=wrt_d.rearrange("(kc p) n -> p kc n", p=128)), b_c5, writes=[b_c5])
            P.dma("sp", lambda e: e.dma_start(out=rbias[:], in_=rbias_d.partition_broadcast(128)), b_c5, writes=[b_c5])
            weg = [sb("weg%d" % i, [128, 8, 512], BF16, st5) for i in range(2)]
            weu = [sb("weu%d" % i, [128, 8, 512], BF16, st5) for i in range(2)]
            wed = [sb("wed%d" % i, [128, 4, 1024], BF16, st5) for i in range(2)]
            b_we = [Buf("we%d" % i) for i in range(2)]
            rl = sb("rl", [128, 16, 36], F32, st5); b_rl = Buf("rl")
            gate = sb("gate", [128, 16, 32], F32, st5); b_gate = Buf("gate")
            PSB = [psum("B%d" % i, [128, 512], F32, st5) for i in range(8)]; b_PSB = [PB("B%d" % i) for i in range(8)]

            def load_expert(ex):
                i = ex % 2
                P.dma("pool", lambda e: e.dma_start(out=weg[i][:, :, :], in_=weg_d[ex].rearrange("(kc p) n -> p kc n", p=128)), b_we[i], writes=[b_we[i]])
                P.dma("pool", lambda e: e.dma_start(out=weu[i][:, :, :], in_=weu_d[ex].rearrange("(kc p) n -> p kc n", p=128)), b_we[i], writes=[b_we[i]])
                P.dma("pool", lambda e: e.dma_start(out=wed[i][:, :, :], in_=wed_d[ex].rearrange("(j p) n -> p j n", p=128)), b_we[i], writes=[b_we[i]])
            load_expert(0)
            load_expert(1)
            with ExitStack() as st5a:
                sq5 = [sb("sq5_%d" % i, [128, D], BF16, st5a) for i in range(2)]; b_sq5 = [Buf("sq5_%d" % i) for i in range(2)]
                h2f = [sb("h2f_%d" % i, [128, D], F32, st5a) for i in range(2)]; b_h2f = [Buf("h2f_%d" % i) for i in range(2)]
                h2Tf = [sb("h2Tf_%d" % i, [128, 8, 128], F32, st5a) for i in range(2)]; b_h2Tf = [Buf("h2Tf_%d" % i) for i in range(2)]
                ss5 = sb("ss5", [128, 16], F32, st5a); b_ss5 = [Buf("ss5_%d" % i) for i in range(16)]
                for tt in range(16):
                    P.dma("sp", lambda e: e.dma_start(out=x1s[:, tt, :], in_=y_d[b, tt * 128:(tt + 1) * 128, :]), b_x1s[tt], writes=[b_x1s[tt]])

                def front5(tt):
                    i2 = tt % 2
                    P.op("act", lambda e: e.activation(out=sq5[i2][:], in_=x1s[:, tt, :], func=AF.Square), reads=[b_x1s[tt]], writes=[b_sq5[i2]])
                    P.op("dve", lambda e: e.tensor_reduce(out=ss5[:, tt:tt + 1], in_=sq5[i2][:], axis=AX.X, op=ALU.add), reads=[b_sq5[i2]], writes=[b_ss5[tt]])
                    P.op("dve", lambda e: e.tensor_scalar(out=ss5[:, tt:tt + 1], in0=ss5[:, tt:tt + 1], scalar1=1.0 / D, scalar2=1e-6, op0=ALU.mult, op1=ALU.add), reads=[b_ss5[tt]], writes=[b_ss5[tt]])
                    P.op("act", lambda e: e.activation(out=ss5[:, tt:tt + 1], in_=ss5[:, tt:tt + 1], func=AF.Sqrt), reads=[b_ss5[tt]], writes=[b_ss5[tt]])
                    P.op("dve", lambda e: e.reciprocal(out=ss5[:, tt:tt + 1], in_=ss5[:, tt:tt + 1]), reads=[b_ss5[tt]], writes=[b_ss5[tt]])
                    P.op("act", lambda e: e.activation(out=h2f[i2][:], in_=x1s[:, tt, :], func=AF.Copy, scale=ss5[:, tt:tt + 1]), reads=[b_x1s[tt], b_ss5[tt]], writes=[b_h2f[i2]])

                def back5(tt):
                    i2 = tt % 2
                    for half in range(2):
                        bk = 2 * i2 + half

                        def tr5(e):
                            for k4 in range(4):
                                kc = half * 4 + k4
                                rr = e.transpose(out=PSB[bk][:, k4 * 128:(k4 + 1) * 128], in_=h2f[i2][:, kc * 128:(kc + 1) * 128], identity=ident_f[:])
                            return rr
                        P.op("pe", tr5, reads=[b_h2f[i2], bC], writes=[b_PSB[bk]])
                        P.op("dve", lambda e: e.tensor_tensor(out=h2Tf[i2][:, half * 4:(half + 1) * 4, :], in0=PSB[bk][:, :].rearrange("p (a t) -> p a t", a=4),
                                                              in1=g2col[:, half * 4:(half + 1) * 4].unsqueeze(2).to_broadcast([128, 4, 128]), op=ALU.mult),
                             reads=[b_PSB[bk], bC], writes=[b_h2Tf[i2]])
                    P.op("pool", lambda e: e.tensor_copy(out=h2T[:, :, tt * 128:(tt + 1) * 128], in_=h2Tf[i2][:]), reads=[b_h2Tf[i2]], writes=[b_h2T])
                    rb = 4 + i2

                    def mm_r(e):
                        for kc in range(8):
                            rr = e.matmul(PSB[rb][:, 0:36], lhsT=h2Tf[i2][:, kc, :], rhs=w_rt[:, kc, :], start=(kc == 0), stop=(kc == 7))
                        return rr
                    P.op("pe", mm_r, reads=[b_h2Tf[i2], b_c5], writes=[b_PSB[rb]])
                    P.op("dve", lambda e: e.tensor_tensor(out=rl[:, tt, :], in0=PSB[rb][:, 0:36], in1=rbias[:], op=ALU.add), reads=[b_PSB[rb], b_c5], writes=[b_rl])
                front5(0)
                for tt in range(16):
                    if tt + 1 < 16:
                        front5(tt + 1)
                    back5(tt)
                def t5(n, shp):
                    return sb(n, shp, F32, st5a)
                gmax = t5("gmax", [128, 16]); gsh = t5("gsh", [128, 16, 4]); gsum = t5("gsum", [128, 16]); gone = t5("gone", [128, 16, 4])
                Em = t5("Em", [128, 16, 32]); m1_ = t5("m1_", [128, 16]); eq1 = t5("eq1", [128, 16, 32]); Em2 = t5("Em2", [128, 16, 32]); m2_ = t5("m2_", [128, 16])
                sel = t5("sel", [128, 16, 32]); esh = t5("esh", [128, 16, 32]); den = t5("den", [128, 16])
                b_r = Buf("routing")
                G = rl[:, :, 0:4]
                E4 = rl[:, :, 4:36].rearrange("p t (g k) -> p t g k", g=4)
                bc = lambda a, n: a[:].unsqueeze(2).to_broadcast([128, 16, n])
                R = lambda fn, eng="dve": P.op(eng, fn, reads=[b_rl], writes=[b_r])
                R(lambda e: e.tensor_reduce(out=gmax[:], in_=G, axis=AX.X, op=ALU.max))
                R(lambda e: e.tensor_tensor(out=gsh[:], in0=G, in1=bc(gmax, 4), op=ALU.subtract))
                R(lambda e: e.activation(out=gsh[:], in_=gsh[:], func=AF.Exp), "act")
                R(lambda e: e.tensor_reduce(out=gsum[:], in_=gsh[:], axis=AX.X, op=ALU.add))
                R(lambda e: e.reciprocal(out=gsum[:], in_=gsum[:]))
                R(lambda e: e.tensor_tensor(out=gone[:], in0=G, in1=bc(gmax, 4), op=ALU.is_equal))
                R(lambda e: e.tensor_scalar(out=gone[:], in0=gone[:], scalar1=1e4, scalar2=-1e4, op0=ALU.mult, op1=ALU.add))
                R(lambda e: e.tensor_tensor(out=Em[:].rearrange("p t (g k) -> p t g k", g=4), in0=E4, in1=gone[:].unsqueeze(3).to_broadcast([128, 16, 4, 8]), op=ALU.add))
                R(lambda e: e.tensor_reduce(out=m1_[:], in_=Em[:], axis=AX.X, op=ALU.max))
                R(lambda e: e.tensor_tensor(out=eq1[:], in0=Em[:], in1=bc(m1_, 32), op=ALU.is_equal))
                R(lambda e: e.scalar_tensor_tensor(out=Em2[:].rearrange("p t k -> p (t k)"), in0=eq1[:].rearrange("p t k -> p (t k)"), scalar=-1e4, in1=Em[:].rearrange("p t k -> p (t k)"), op0=ALU.mult, op1=ALU.add))
                R(lambda e: e.tensor_reduce(out=m2_[:], in_=Em2[:], axis=AX.X, op=ALU.max))
                R(lambda e: e.tensor_tensor(out=sel[:], in0=Em[:], in1=bc(m2_, 32), op=ALU.is_ge))
                R(lambda e: e.tensor_tensor(out=esh[:], in0=Em[:], in1=bc(m1_, 32), op=ALU.subtract))
                R(lambda e: e.activation(out=esh[:], in_=esh[:], func=AF.Exp), "act")
                R(lambda e: e.tensor_tensor(out=sel[:], in0=sel[:], in1=esh[:], op=ALU.mult))
                R(lambda e: e.tensor_reduce(out=den[:], in_=sel[:], axis=AX.X, op=ALU.add))
                R(lambda e: e.reciprocal(out=den[:], in_=den[:]))
                R(lambda e: e.tensor_tensor(out=den[:], in0=den[:], in1=gsum[:], op=ALU.mult))
                P.op("dve", lambda e: e.tensor_tensor(out=gate[:], in0=sel[:], in1=bc(den, 32), op=ALU.mult), reads=[b_r], writes=[b_gate])
                if dbg and b == 0:
                    P.dma("sp", lambda e: e.dma_start(out=dbg_d["gate"][:, :], in_=gate[:].rearrange("p t k -> p (t k)")), b_gate, reads=[b_gate], is_out=True)
            P.barrier()
            with ExitStack() as st5b:
                sgt = [sb("sgt%d" % i, [128, 512], F32, st5b) for i in range(2)]; b_sgt = [Buf("sgt%d" % i) for i in range(2)]
                hid = [sb("hid%d" % i, [128, 4, 512], BF16, st5b) for i in range(2)]; b_hid = [Buf("hid%d" % i) for i in range(2)]
                nex = NEXP if stage >= 6 else 2
                yc = 0
                def emit_gu(ex, tq):
                    wi = ex % 2
                    qsl = slice(tq * 512, (tq + 1) * 512)
                    hi = (ex * 4 + tq) % 2
                    for j in range(4):
                        jsl = slice(j * 128, (j + 1) * 128)
                        pgb = j % 2; pub = 2 + j % 2

                        def mm_gu(e):
                            for kc in range(8):
                                e.matmul(PSB[pgb][:, :], lhsT=weg[wi][:, kc, jsl], rhs=h2T[:, kc, qsl], start=(kc == 0), stop=(kc == 7))
                            for kc in range(8):
                                rr = e.matmul(PSB[pub][:, :], lhsT=weu[wi][:, kc, jsl], rhs=h2T[:, kc, qsl], start=(kc == 0), stop=(kc == 7))
                            return rr
                        P.op("pe", mm_gu, reads=[b_we[wi], b_h2T], writes=[b_PSB[pgb], b_PSB[pub]])
                        P.op("act", lambda e: e.activation(out=sgt[j % 2][:], in_=PSB[pgb][:, :], func=AF.Silu), reads=[b_PSB[pgb]], writes=[b_sgt[j % 2]])
                        P.op("dve", lambda e: e.tensor_tensor(out=hid[hi][:, j, :], in0=PSB[pub][:, :], in1=sgt[j % 2][:], op=ALU.mult), reads=[b_PSB[pub], b_sgt[j % 2]], writes=[b_hid[hi]])

                def emit_y(ex, tq):
                    wi = ex % 2
                    hi = (ex * 4 + tq) % 2
                    for t4 in range(4):
                        tt = tq * 4 + t4
                        for half in range(2):
                            yb = 4 + ycnt[0] % 4
                            ycnt[0] += 1

                            def mm_y(e):
                                for j in range(4):
                                    rr = e.matmul(PSB[yb][:, :], lhsT=hid[hi][:, j, t4 * 128:(t4 + 1) * 128], rhs=wed[wi][:, j, half * 512:(half + 1) * 512], start=(j == 0), stop=(j == 3))
                                return rr
                            P.op("pe", mm_y, reads=[b_hid[hi], b_we[wi]], writes=[b_PSB[yb]])
                            P.op("dve", lambda e: e.scalar_tensor_tensor(out=x1s[:, tt, half * 512:(half + 1) * 512], in0=PSB[yb][:, :], scalar=gate[:, tt, ex:ex + 1],
                                                                         in1=x1s[:, tt, half * 512:(half + 1) * 512], op0=ALU.mult, op1=ALU.add),
                                 reads=[b_PSB[yb], b_gate, b_x1s[tt]], writes=[b_x1s[tt]])

                ycnt = [0]
                units = [(ex, tq) for ex in range(nex) for tq in range(4)]
                emit_gu(*units[0])
                for k, (ex, tq) in enumerate(units):
                    if k + 1 < len(units):
                        emit_gu(*units[k + 1])
                    emit_y(ex, tq)
                    if tq == 3 and ex + 2 < nex:
                        load_expert(ex + 2)
                for tt in range(16):
                    P.dma("sp", lambda e: e.dma_start(out=y_d[b, tt * 128:(tt + 1) * 128, :], in_=x1s[:, tt, :]), b_x1s[tt], reads=[b_x1s[tt]], is_out=True)
        P.barrier()

    print("total ops", P.nops)
    if P.trace_lines is not None:
        build_program.trace_lines = P.trace_lines
    P.finish()
    return nc


def host_inputs(inputs):
    f = lambda a: np.ascontiguousarray(np.asarray(a, dtype=np.float32))
    com = {}
    com["w_in"] = f(inputs["w_in"][0])
    com["g1col"] = f(inputs["norm1_gain"][0].reshape(8, 128).T)
    com["g2col"] = f(inputs["norm2_gain"][0].reshape(8, 128).T)
    qg = inputs["q_norm_gain"][0]; kg = inputs["k_norm_gain"][0]
    com["qkgc"] = f(np.stack([np.tile(v, 2) for g in range(3) for v in (qg[g], kg[g])], axis=1))
    bt = _bucket_table()
    tab = np.asarray(inputs["rel_bias_table"], dtype=np.float32)
    jj = np.arange(128)[:, None]; ii = np.arange(128)[None, :]
    rel_prev = ii + 128 - jj
    rel_cur = ii - jj
    biasg = np.zeros((128, 12, 2, 128), np.float32)
    for g in range(3):
        d = DILS[g]
        bp = bt[np.clip(rel_prev, 0, 128) * d]
        bc = bt[np.clip(rel_cur, 0, 128) * d]
        for hh in range(4):
            biasg[:, g * 4 + hh, 0, :] = tab[bp, g * 4 + hh]
            biasg[:, g * 4 + hh, 1, :] = tab[bc, g * 4 + hh]
    com["biasg"] = f(biasg.reshape(128, 12 * 256))
    am = np.zeros((128, 2, 128), np.float32)
    am[:, 0, :] = (rel_prev <= 128)
    am[:, 1, :] = (rel_cur >= 0)
    com["amask"] = f(am.reshape(128, 256))
    com["ident"] = np.eye(128, dtype=np.float32)
    ch4 = lambda v: np.asarray(v, np.float32).reshape(4, 128).T
    mu = np.asarray(inputs["rwkv_shift_mu"][0], np.float32)
    gb_mu = np.zeros((128, 1), np.float32); gb_mu[:32, 0] = mu[1792:1824]
    cols = [ch4(mu[0:512]), ch4(mu[512:1024]), ch4(mu[1024:1536]), mu[1536:1664].reshape(128, 1), mu[1664:1792].reshape(128, 1), gb_mu,
            ch4(inputs["rwkv_w0"][0]), ch4(inputs["rwkv_a0"][0]), ch4(inputs["rwkv_k_k"][0]), ch4(inputs["rwkv_k_a"][0]),
            ch4(inputs["rwkv_r_k"][0].reshape(-1)), ch4(inputs["rwkv_ln_w"][0]), ch4(inputs["rwkv_ln_b"][0])]
    com["rwcol"] = f(np.concatenate(cols, axis=1))
    com["lora"] = f(np.concatenate([inputs["rwkv_w_up"][0], inputs["rwkv_a_up"][0]], axis=0))
    com["gup"] = f(inputs["rwkv_g_up"][0])
    jj = np.arange(128)[:, None]; tt = np.arange(128)[None, :]
    same = (jj // 64) == (tt // 64)
    su = (same & (jj < tt)).astype(np.float32); uu = (same & (jj <= tt)).astype(np.float32); sl = (same & (jj > tt)).astype(np.float32)
    bo = same.astype(np.float32)
    rs = np.ones((128, 1024), np.float32); rs[:, ::64] = 0.0
    com["rwmask"] = f(np.concatenate([su, uu, su, uu, sl, bo, rs], axis=1))
    com["w_out"] = f(inputs["w_out"][0])
    com["w_att_out"] = f(inputs["w_att_out"][0])
    com["w_rwkv_out"] = f(inputs["w_rwkv_out"][0])
    com["w_router"] = f(np.concatenate([inputs["w_group_router"][0], inputs["w_expert_router"][0]], axis=1))
    com["b_router"] = f(np.concatenate([inputs["b_group_router"][0], inputs["b_expert_router"][0]]))
    com["w_expert_gate"] = f(inputs["w_expert_gate"][0])
    com["w_expert_up"] = f(inputs["w_expert_up"][0])
    com["w_expert_down"] = f(inputs["w_expert_down"][0])
    return com


def kernel(**inputs):
    x = np.asarray(inputs["x"], dtype=np.float32)
    com = host_inputs(inputs)
    nc = build_program()
    in_maps = []
    for c in range(N_CORES):
        m = dict(com)
        m["x"] = np.ascontiguousarray(x[c * NSEQ:(c + 1) * NSEQ])
        in_maps.append(m)
    res = run_bass_kernel_spmd(nc, in_maps, core_ids=list(range(N_CORES)))
    return np.concatenate([r["y"] for r in res.results], axis=0)
```
